# Optimizing a Trainium2 kernel written in Bass

```python
import math
import jax, jax.numpy as jnp
from jax import lax
import numpy as np

D_MODEL = 1024
BATCH = 2
SEQ = 8192
DEPTH = 2

CHUNK = 64
CONV_WIDTH = D_MODEL // 2
CONV_KERNEL = 31
ATT_HEADS = 4
ATT_HEAD_DIM = 64
ATT_VALUE_DIM = 2 * ATT_HEAD_DIM
ATT_WIDTH = ATT_HEADS * ATT_VALUE_DIM
MIX_WIDTH = CONV_WIDTH + ATT_WIDTH
QK_WIDTH = 2 * ATT_HEADS * ATT_HEAD_DIM
MIX_IN = 2 * CONV_WIDTH + 2 * QK_WIDTH + ATT_WIDTH
OFF_Q = 2 * CONV_WIDTH
OFF_K = OFF_Q + QK_WIDTH
OFF_V = OFF_K + QK_WIDTH
ROPE_THETA = 10000.0
Q_BLOCK = 128
N_GROUPS = 4
EXPERTS_PER_GROUP = 8
N_EXPERTS = N_GROUPS * EXPERTS_PER_GROUP
TOP_K_FINE = 2
D_EXPERT = D_MODEL // 2
MOE_BLOCK = 128
LN_EPS = 1e-5
DEEPNORM_ALPHA = (2.0 * DEPTH) ** 0.25
DEEPNORM_BETA = (8.0 * DEPTH) ** -0.25

kernel_name = "hymba_conformer_diffattn_hiermoe_deepnorm"


def _layernorm(x, g, b):
    xf = x.astype(jnp.float32)
    mu = jnp.mean(xf, axis=-1, keepdims=True)
    var = jnp.mean(jnp.square(xf - mu), axis=-1, keepdims=True)
    return ((xf - mu) * lax.rsqrt(var + LN_EPS) * g.astype(jnp.float32) + b.astype(jnp.float32)).astype(x.dtype)


def _rmsnorm(x, g):
    xf = x.astype(jnp.float32)
    ms = jnp.mean(jnp.square(xf), axis=-1, keepdims=True)
    return (xf * lax.rsqrt(ms + LN_EPS) * g.astype(jnp.float32)).astype(x.dtype)


def _rope_tables(seq):
    half = ATT_HEAD_DIM // 2
    inv_freq = 1.0 / (ROPE_THETA ** (jnp.arange(half, dtype=jnp.float32) * 2.0 / ATT_HEAD_DIM))
    ang = jnp.arange(seq, dtype=jnp.float32)[:, None] * inv_freq[None, :]
    return jnp.cos(ang)[:, None, :], jnp.sin(ang)[:, None, :]


def _rope(x, cos, sin):
    half = ATT_HEAD_DIM // 2
    xf = x.astype(jnp.float32)
    x1, x2 = xf[..., :half], xf[..., half:]
    return jnp.concatenate([x1 * cos - x2 * sin, x2 * cos + x1 * sin], axis=-1).astype(x.dtype)


def _causal_depthwise_conv(u, w, b):
    y = lax.conv_general_dilated(u, w.astype(u.dtype), window_strides=(1,),
                                 padding=[(CONV_KERNEL - 1, 0)],
                                 dimension_numbers=("NWC", "WIO", "NWC"),
                                 feature_group_count=u.shape[-1])
    return y + b


def _diff_attention(q, k, v, lam):
    bsz, seq = q.shape[0], q.shape[1]
    n_blocks = seq // Q_BLOCK
    scale = ATT_HEAD_DIM ** -0.5
    kf = k.astype(jnp.float32)
    vf = v.astype(jnp.float32)
    k_chunk = jnp.arange(seq) // CHUNK
    qb_all = jnp.swapaxes(q.reshape(bsz, n_blocks, Q_BLOCK, 2 * ATT_HEADS, ATT_HEAD_DIM), 0, 1)

    def one_block(args):
        qb, bi = args
        s = jnp.einsum("bqhd,bkhd->bhqk", qb.astype(jnp.float32), kf) * scale
        q_chunk = (bi * Q_BLOCK + jnp.arange(Q_BLOCK)) // CHUNK
        allowed = k_chunk[None, :] <= q_chunk[:, None]
        p = jax.nn.softmax(jnp.where(allowed[None, None], s, -jnp.inf), axis=-1)
        p = p.reshape(bsz, ATT_HEADS, 2, Q_BLOCK, seq)
        a = p[:, :, 0] - lam * p[:, :, 1]
        return jnp.einsum("bhqk,bkhe->bqhe", a, vf).astype(v.dtype)

    o = lax.map(one_block, (qb_all, jnp.arange(n_blocks)))
    return jnp.swapaxes(o, 0, 1).reshape(bsz, seq, ATT_HEADS, ATT_VALUE_DIM)


def _mixer(x, w_in, b_in, conv_w, conv_b, conv_ln_g, conv_ln_b,
           lam_q1, lam_k1, lam_q2, lam_k2, subln_g, w_out, cos, sin, lam_init):
    bsz, seq, _ = x.shape
    proj = x @ w_in + b_in
    u = proj[..., :CONV_WIDTH] * jax.nn.sigmoid(proj[..., CONV_WIDTH:OFF_Q])
    u = _causal_depthwise_conv(u, conv_w, conv_b)
    u = jax.nn.silu(_layernorm(u, conv_ln_g, conv_ln_b))
    q = _rope(proj[..., OFF_Q:OFF_K].reshape(bsz, seq, 2 * ATT_HEADS, ATT_HEAD_DIM), cos, sin)
    k = _rope(proj[..., OFF_K:OFF_V].reshape(bsz, seq, 2 * ATT_HEADS, ATT_HEAD_DIM), cos, sin)
    v = proj[..., OFF_V:].reshape(bsz, seq, ATT_HEADS, ATT_VALUE_DIM)
    lam = (jnp.exp(jnp.sum(lam_q1.astype(jnp.float32) * lam_k1.astype(jnp.float32)))
           - jnp.exp(jnp.sum(lam_q2.astype(jnp.float32) * lam_k2.astype(jnp.float32))) + lam_init)
    o = _diff_attention(q, k, v, lam)
    o = _rmsnorm(o, subln_g) * (1.0 - lam_init)
    merged = jnp.concatenate([u, o.reshape(bsz, seq, ATT_WIDTH)], axis=-1)
    return merged @ w_out


def _hier_moe(x, w_rg, b_rg, w_re, b_re, w_gate_e, w_up_e, w_down_e):
    bsz, seq, d = x.shape
    n_tok = bsz * seq
    xt = x.reshape(n_tok, d)
    pg = jax.nn.softmax((xt @ w_rg + b_rg).astype(jnp.float32), axis=-1)
    grp = jnp.argmax(pg, axis=-1)
    gate_g = jnp.max(pg, axis=-1)
    el = (xt @ w_re + b_re).astype(jnp.float32).reshape(n_tok, N_GROUPS, EXPERTS_PER_GROUP)
    el_g = jnp.take_along_axis(el, grp[:, None, None], axis=1)[:, 0]
    top_v, top_i = lax.top_k(el_g, TOP_K_FINE)
    wts = gate_g[:, None] * jax.nn.softmax(top_v, axis=-1)
    n_assign = n_tok * TOP_K_FINE
    eid = (grp[:, None] * EXPERTS_PER_GROUP + top_i).reshape(n_assign).astype(jnp.int32)
    tok = jnp.repeat(jnp.arange(n_tok, dtype=jnp.int32), TOP_K_FINE)
    w_a = wts.reshape(n_assign)
    order = jnp.argsort(eid)
    eid_s, tok_s, w_s = eid[order], tok[order], w_a[order]
    counts = jnp.zeros((N_EXPERTS,), jnp.int32).at[eid].add(1)
    padded = ((counts + MOE_BLOCK - 1) // MOE_BLOCK) * MOE_BLOCK
    pad_end = jnp.cumsum(padded)
    pad_start = pad_end - padded
    start = jnp.cumsum(counts) - counts
    dest = pad_start[eid_s] + (jnp.arange(n_assign, dtype=jnp.int32) - start[eid_s])
    n_blocks = -(-n_assign // MOE_BLOCK) + N_EXPERTS
    n_rows = n_blocks * MOE_BLOCK
    tok_pad = jnp.zeros((n_rows,), jnp.int32).at[dest].set(tok_s)
    w_pad = jnp.zeros((n_rows,), w_s.dtype).at[dest].set(w_s)
    blk_e = jnp.minimum(jnp.searchsorted(pad_end, jnp.arange(n_blocks, dtype=jnp.int32) * MOE_BLOCK,
                                         side="right"), N_EXPERTS - 1).astype(jnp.int32)

    def expert_block(args):
        tb, e = args
        xb = xt[tb]
        h = jax.nn.silu(xb @ w_gate_e[e]) * (xb @ w_up_e[e])
        return h @ w_down_e[e]

    yb = lax.map(expert_block, (tok_pad.reshape(n_blocks, MOE_BLOCK), blk_e))
    y = yb.reshape(n_rows, d) * w_pad[:, None].astype(x.dtype)
    out = jnp.zeros((n_tok, d), x.dtype).at[tok_pad].add(y)
    return out.reshape(bsz, seq, d)


def setup_inputs(seed: int = 0) -> dict:
    key = jax.random.key(seed)
    ks = jax.random.split(key, 24)
    nrm = jax.random.normal
    f32 = jnp.float32
    x = nrm(ks[0], (BATCH, SEQ, D_MODEL), f32)
    s_in = D_MODEL ** -0.5
    w_in = jnp.concatenate([
        nrm(ks[1], (DEPTH, D_MODEL, OFF_V), f32) * s_in,
        nrm(ks[2], (DEPTH, D_MODEL, ATT_WIDTH), f32) * s_in * DEEPNORM_BETA,
    ], axis=-1)
    b_in = 0.02 * nrm(ks[3], (DEPTH, MIX_IN), f32)
    conv_w = nrm(ks[4], (DEPTH, CONV_KERNEL, 1, CONV_WIDTH), f32) * CONV_KERNEL ** -0.5
    conv_b = 0.02 * nrm(ks[5], (DEPTH, CONV_WIDTH), f32)
    conv_ln_g = 1.0 + 0.05 * nrm(ks[6], (DEPTH, CONV_WIDTH), f32)
    conv_ln_b = 0.02 * nrm(ks[7], (DEPTH, CONV_WIDTH), f32)
    lam_q1 = 0.1 * nrm(ks[8], (DEPTH, ATT_HEAD_DIM), f32)
    lam_k1 = 0.1 * nrm(ks[9], (DEPTH, ATT_HEAD_DIM), f32)
    lam_q2 = 0.1 * nrm(ks[10], (DEPTH, ATT_HEAD_DIM), f32)
    lam_k2 = 0.1 * nrm(ks[11], (DEPTH, ATT_HEAD_DIM), f32)
    subln_g = 1.0 + 0.05 * nrm(ks[12], (DEPTH, ATT_VALUE_DIM), f32)
    w_out = nrm(ks[13], (DEPTH, MIX_WIDTH, D_MODEL), f32) * MIX_WIDTH ** -0.5 * DEEPNORM_BETA
    ln1_g = 1.0 + 0.05 * nrm(ks[14], (DEPTH, D_MODEL), f32)
    ln1_b = 0.02 * nrm(ks[15], (DEPTH, D_MODEL), f32)
    w_rg = nrm(ks[16], (DEPTH, D_MODEL, N_GROUPS), f32) * s_in
    b_rg = 0.01 * nrm(ks[17], (DEPTH, N_GROUPS), f32)
    w_re = nrm(ks[18], (DEPTH, D_MODEL, N_EXPERTS), f32) * s_in
    b_re = 0.01 * nrm(ks[19], (DEPTH, N_EXPERTS), f32)
    w_gate_e = nrm(ks[20], (DEPTH, N_EXPERTS, D_MODEL, D_EXPERT), f32) * s_in
    w_up_e = nrm(ks[21], (DEPTH, N_EXPERTS, D_MODEL, D_EXPERT), f32) * s_in
    w_down_e = nrm(ks[22], (DEPTH, N_EXPERTS, D_EXPERT, D_MODEL), f32) * D_EXPERT ** -0.5 * DEEPNORM_BETA
    k2a, k2b = jax.random.split(ks[23])
    ln2_g = 1.0 + 0.05 * nrm(k2a, (DEPTH, D_MODEL), f32)
    ln2_b = 0.02 * nrm(k2b, (DEPTH, D_MODEL), f32)
    return {"x": x, "w_in": w_in, "b_in": b_in, "conv_w": conv_w, "conv_b": conv_b,
            "conv_ln_g": conv_ln_g, "conv_ln_b": conv_ln_b,
            "lam_q1": lam_q1, "lam_k1": lam_k1, "lam_q2": lam_q2, "lam_k2": lam_k2,
            "subln_g": subln_g, "w_out": w_out, "ln1_g": ln1_g, "ln1_b": ln1_b,
            "w_rg": w_rg, "b_rg": b_rg, "w_re": w_re, "b_re": b_re,
            "w_gate_e": w_gate_e, "w_up_e": w_up_e, "w_down_e": w_down_e,
            "ln2_g": ln2_g, "ln2_b": ln2_b}


def reference(x, w_in, b_in, conv_w, conv_b, conv_ln_g, conv_ln_b,
              lam_q1, lam_k1, lam_q2, lam_k2, subln_g, w_out, ln1_g, ln1_b,
              w_rg, b_rg, w_re, b_re, w_gate_e, w_up_e, w_down_e, ln2_g, ln2_b):
    cos, sin = _rope_tables(x.shape[1])
    for l in range(DEPTH):
        lam_init = 0.8 - 0.6 * math.exp(-0.3 * l)
        m = _mixer(x, w_in[l], b_in[l], conv_w[l], conv_b[l], conv_ln_g[l], conv_ln_b[l],
                   lam_q1[l], lam_k1[l], lam_q2[l], lam_k2[l], subln_g[l], w_out[l],
                   cos, sin, lam_init)
        x = _layernorm(DEEPNORM_ALPHA * x + m, ln1_g[l], ln1_b[l])
        f = _hier_moe(x, w_rg[l], b_rg[l], w_re[l], b_re[l], w_gate_e[l], w_up_e[l], w_down_e[l])
        x = _layernorm(DEEPNORM_ALPHA * x + f, ln2_g[l], ln2_b[l])
    return x
```

```python
import math
from contextlib import ExitStack

import numpy as np
import concourse.bass as bass
import concourse.mybir as mybir
from concourse.bass_utils import run_bass_kernel_spmd

F32 = mybir.dt.float32
BF16 = mybir.dt.bfloat16
I32 = mybir.dt.int32
ALU = mybir.AluOpType
AF = mybir.ActivationFunctionType
AX = mybir.AxisListType

D = 1024
NB = 16
T = NB * 128
DEPTH = 2
CAP = 256
NE = 32
ALPHA = (2.0 * DEPTH) ** 0.25
LN_EPS = 1e-5
BIG = float(NE * CAP)
NEG = -30000.0

ENGS = ("pe", "act", "dve", "pool", "sp")


class Op:
    __slots__ = ("eng", "fn", "deps", "dma", "signal", "tok", "inc")

    def __init__(self, eng, fn, dma, inc):
        self.eng, self.fn, self.dma, self.inc = eng, fn, dma, inc
        self.deps = []
        self.signal = False
        self.tok = None


class Prog:
    def __init__(self):
        self.ops = []
        self.lastw = {}
        self.readers = {}
        self.stream_last = {}
        self.last_on_eng = {}

    def begin_record(self):
        self.rec = []

    def end_record(self):
        r, self.rec = self.rec, None
        return r

    def replay_interleaved(self, lists):
        n = max(len(x) for x in lists)
        for j in range(n):
            for x in lists:
                if j < len(x):
                    a, kw = x[j]
                    self.add(*a, **kw)

    def add(self, eng, fn, reads=(), writes=(), dma=None, inc=16):
        if getattr(self, "rec", None) is not None:
            self.rec.append(((eng, fn), dict(reads=list(reads), writes=list(writes), dma=dma, inc=inc)))
            return None
        op = Op(eng, fn, dma, inc)
        deps = set()
        for k in reads:
            w = self.lastw.get(k)
            if w is not None:
                deps.add(w)
        for k in writes:
            w = self.lastw.get(k)
            if w is not None:
                deps.add(w)
            for r in self.readers.get(k, ()):
                deps.add(r)
        if dma is not None:
            p = self.stream_last.get(dma)
            if p is not None:
                deps.add(p)
            self.stream_last[dma] = op
        for k in writes:
            self.lastw[k] = op
            self.readers[k] = []
        for k in reads:
            if k not in writes:
                self.readers.setdefault(k, []).append(op)
        deps.discard(op)
        op.deps = list(deps)
        self.ops.append(op)
        self.last_on_eng[eng] = op
        return op

    def barrier(self):
        pend = list(self.last_on_eng.values()) + [v for k, v in self.stream_last.items()
                                                  if not (k.startswith("cc") and k.endswith("_1"))]
        for e in ENGS:
            op = Op(e, None, None, 0)
            op.deps = [d for d in pend]
            self.ops.append(op)

    def emit(self, nc, block, es):
        for op in self.ops:
            for d in op.deps:
                if d.eng == "pe" and op.eng == "pe" and d.dma is None and op.dma is None:
                    continue
                d.signal = True
        sems = {e: es.enter_context(nc.semaphore("s_" + e)) for e in ENGS}
        ssem = {}
        cnt = {e: 0 for e in ENGS}
        scnt = {}
        for op in self.ops:
            if op.fn is None:
                continue
            if op.dma is not None:
                if op.dma not in ssem:
                    ssem[op.dma] = es.enter_context(nc.semaphore("d_" + op.dma))
                    scnt[op.dma] = 0
                scnt[op.dma] += op.inc
                op.tok = (ssem[op.dma], scnt[op.dma])
                op.signal = True
            elif op.signal:
                cnt[op.eng] += 1
                op.tok = (sems[op.eng], cnt[op.eng])
        per = {e: [o for o in self.ops if o.eng == e] for e in ENGS}

        def run(eng_name, eng):
            waited = {}
            for op in per[eng_name]:
                for d in op.deps:
                    if d.tok is None:
                        continue
                    if d.eng == "pe" and eng_name == "pe" and d.dma is None:
                        continue
                    s, v = d.tok
                    if waited.get(id(s), 0) >= v:
                        continue
                    eng.wait_ge(s, v)
                    waited[id(s)] = v
                if op.fn is None:
                    continue
                ins = op.fn(eng)
                if op.signal:
                    if op.dma is not None and op.dma.startswith("cc"):
                        ins.then_inc(op.tok[0])
                    else:
                        ins.then_inc(op.tok[0], op.inc if op.dma is not None else 1)

        block.tensor(lambda e: run("pe", e))
        block.scalar(lambda e: run("act", e))
        block.vector(lambda e: run("dve", e))
        block.gpsimd(lambda e: run("pool", e))
        block.sync(lambda e: run("sp", e))


def build(n_layers=DEPTH, stop_after=None, dbg=None, ne_decl=NE):
    nc = bass.Bass("TRN2", target_bir_lowering=False)
    P = Prog()
    es = ExitStack()

    def din(name, shape, dt=F32):
        return nc.dram_tensor(name, list(shape), dt, kind="ExternalInput")

    x_in = din("x", [NB, 128, D])
    w_in = din("w_in", [DEPTH, D, 3584])
    bfm = din("bfm", [DEPTH, 128, 24])
    bv = din("bv", [DEPTH, 128, 512])
    cosT = din("cosT", [128, T])
    sinT = din("sinT", [128, T])
    convp = din("convp", [DEPTH, 128, 4, 34])
    lamv = din("lamv", [DEPTH, 128, 4, 64])
    gsub = din("gsub", [DEPTH, 128, 128])
    w_out = din("w_out", [DEPTH, D, D])
    lnp = din("lnp", [DEPTH, 4, 128, D])
    w_r = din("w_r", [DEPTH, D, 36])
    b_r = din("b_r", [DEPTH, 128, 36])
    w_g = din("w_gate_e", [DEPTH, ne_decl, D, 512])
    w_u = din("w_up_e", [DEPTH, ne_decl, D, 512])
    w_d = din("w_down_e", [DEPTH, ne_decl, 512, D])
    ident_in = din("ident", [128, 128])
    tri_in = din("tri", [128, 128])
    ec_in = din("ec", [128, NE])
    maskb_in = din("maskb", [128, 4, 128])
    hcoef_in = din("hcoef", [128, 4])
    out = nc.dram_tensor("out", [NB, 128, D], F32, kind="ExternalOutput")
    dbg_out = {}
    if dbg is not None:
        for (dn, dfn, dshape, ddt) in dbg:
            dbg_out[dn] = nc.dram_tensor("dbg_" + dn, list(dshape), ddt, kind="ExternalOutput")

    KTb = [nc.dram_tensor("KTb%d" % a, [256, T], BF16) for a in range(2)]
    KTg = [nc.dram_tensor("KTg%d" % a, [1024, T], BF16) for a in range(2)]
    Vb = [nc.dram_tensor("Vb%d" % a, [T, 256], BF16) for a in range(2)]
    Vg = [nc.dram_tensor("Vg%d" % a, [4 * T, 256], BF16) for a in range(2)]
    Hb = nc.dram_tensor("Hb", [512, 512], F32)
    Hg = nc.dram_tensor("Hg", [2048, 512], F32)
    xs = nc.dram_tensor("xs", [NE * CAP + 1, D], BF16)
    ys = nc.dram_tensor("ys", [NE * CAP * 2 + 2, 512], F32)

    def sb(name, shape, dt):
        return es.enter_context(nc.sbuf_tensor("sb_" + name, list(shape), dt))

    X = sb("X", [128, NB, D], F32)
    R1 = sb("R1", [128, 16384], BF16)
    R2 = sb("R2", [128, 8192], F32)
    R3 = sb("R3", [128, 8192], BF16)
    R4 = sb("R4", [128, 8192], F32)
    PT = sb("PT", [128, 4, 512], BF16)
    SC = sb("SC", [128, 3072], F32)
    ident = sb("ident", [128, 128], F32)
    identb = sb("identb", [128, 128], BF16)
    trib = sb("trib", [128, 128], BF16)
    onesb = sb("onesb", [128, 128], BF16)
    onesf = sb("onesf", [128, 128], F32)
    ecs = sb("ecs", [128, NE], F32)
    maskb = sb("maskb", [128, 4, 128], BF16)
    hcoef = sb("hcoef", [128, 4], F32)
    bfm_s = sb("bfm_s", [128, 24], F32)
    convp_s = sb("convp_s", [128, 4, 34], F32)
    gsub_s = sb("gsub_s", [128, 128], F32)
    br_s = sb("br_s", [128, 36], F32)
    wr_s = sb("wr_s", [128, 8, 36], F32)
    small = sb("small", [128, 64], F32)
    epsT = sb("epsT", [128, 1], F32)
    lnsm = sb("lnsm", [128, 2, 20], F32)
    destI = sb("destI", [128, NB, 2], I32)
    destG = sb("destG", [128, NB, 4], I32)
    wgt = sb("wgt", [128, NB, 2], F32)
    selB = sb("selB", [128, NB, NE], BF16)

    Y1 = sb("Y1", [128, 512], F32)
    zt = sb("zt", [128, D], BF16)
    ps = [es.enter_context(nc.psum_tensor("ps%d" % i, [128, 512], F32)) for i in range(8)]

    xT = R1[:, :].rearrange("p (k t) -> p k t", k=8)
    mergedT = xT
    uT = R2[:, :].rearrange("p (c t) -> p c t", c=4)
    KTh = [R2[:, :].bitcast(BF16)[:, s * 8192:(s + 1) * 8192].rearrange("p (r t) -> p r t", r=4)
           for s in range(2)]
    QT = R3[:, :].rearrange("p (h t) -> p h t", h=4)
    R4b = R4[:, :].bitcast(BF16)
    Vh = R4b[:, 0:64 * 129].rearrange("p (j e) -> p j e", e=129)
    Win = [R4b[:, s * 2048:(s + 1) * 2048].rearrange("p (k n) -> p k n", k=8) for s in range(3)]
    cosA = R4[:, 3072:5120]
    sinA = R4[:, 5120:7168]
    HG = R4[:, 3072:5120].rearrange("p (r c) -> p r c", r=4)
    ext = R4[:, 5120:7680].rearrange("p (i c) -> p i c", i=16)
    tmpA = R4[:, 7680:8192]
    lam_s = R4[:, 7936:8192].rearrange("p (a d) -> p a d", a=4)
    Qph = SC[:, 0:2048].bitcast(BF16).rearrange("p (m t) -> p m t", m=2)
    scr = [SC[:, 2048 + i * 512: 2048 + (i + 1) * 512] for i in range(2)]
    scq = [SC[:, i * 512:(i + 1) * 512] for i in range(4)]

    bank_ctr = [0]

    def bank():
        b = bank_ctr[0] % 8
        bank_ctr[0] += 1
        return b

    P.add("sp", lambda e: e.dma_start(out=ident[:], in_=ident_in[:, :]), writes=["ident"], dma="c0")
    P.add("pool", lambda e: e.dma_start(out=identb[:], in_=ident_in[:, :]), writes=["identb"], dma="c1")
    P.add("pool", lambda e: e.dma_start(out=trib[:], in_=tri_in[:, :]), writes=["trib"], dma="c1")
    P.add("pool", lambda e: e.dma_start(out=maskb[:], in_=maskb_in[:, :, :]), writes=["maskb"], dma="c1")
    P.add("sp", lambda e: e.dma_start(out=ecs[:], in_=ec_in[:, :]), writes=["ecs"], dma="c0")
    P.add("sp", lambda e: e.dma_start(out=hcoef[:], in_=hcoef_in[:, :]), writes=["hcoef"], dma="c0")
    P.add("pool", lambda e: e.memset(onesb[:], 1.0), writes=["onesb"])
    P.add("pool", lambda e: e.memset(onesf[:], 1.0 / 512.0), writes=["onesf"])
    P.add("pool", lambda e: e.memset(epsT[:], LN_EPS), writes=["epsT"])
    P.add("pool", lambda e: e.memset(Y1[:, :], 0.0), writes=["y1"])
    P.add("sp", lambda e: e.dma_start(out=ys[NE * CAP * 2:NE * CAP * 2 + 2, :], in_=Y1[0:2, 0:512]), reads=["y1"], writes=["ys"], dma="c0")
    for i in range(NB):
        P.add("sp", lambda e, i=i: e.dma_start(out=X[:, i, :], in_=x_in[i, :, :]), writes=["X%d" % i], dma="xl%d" % (i % 4))
    P.barrier()

    def ln_block(l, i, which, src_key_extra=()):
        g_t = lnG[which]
        b_t = lnB[which]
        xi = X[:, i, :]
        q = i % 2
        sm_ = lnsm[:, q, :]
        kq = "ln%d" % q
        st = sm_[:, 0:12].rearrange("p (a b) -> p a b", a=2)
        P.add("dve", lambda e: e.bn_stats(out=st[:, 0, :], in_=X[:, i, 0:512]), reads=["X%d" % i], writes=[kq])
        P.add("dve", lambda e: e.bn_stats(out=st[:, 1, :], in_=X[:, i, 512:1024]), reads=["X%d" % i], writes=[kq])
        P.add("dve", lambda e: e.bn_aggr(out=sm_[:, 12:14], in_=st), reads=[kq], writes=[kq])
        P.add("act", lambda e: e.activation(out=sm_[:, 14:15], in_=sm_[:, 13:14], func=AF.Ln, bias=epsT[:, 0:1]),
              reads=[kq, "epsT"], writes=[kq])
        P.add("act", lambda e: e.activation(out=sm_[:, 15:16], in_=sm_[:, 14:15], func=AF.Exp, scale=-0.5),
              reads=[kq], writes=[kq])
        P.add("dve", lambda e: e.scalar_tensor_tensor(out=sm_[:, 16:17], in0=sm_[:, 12:13], scalar=-1.0, in1=sm_[:, 15:16],
                                                      op0=ALU.mult, op1=ALU.mult),
              reads=[kq], writes=[kq])
        P.add("act", lambda e: e.activation(out=xi, in_=xi, func=AF.Identity, scale=sm_[:, 15:16], bias=sm_[:, 16:17]),
              reads=["X%d" % i, kq], writes=["X%d" % i])
        P.add("dve", lambda e: e.tensor_tensor(out=xi, in0=xi, in1=g_t, op=ALU.mult),
              reads=["X%d" % i, "lnG"], writes=["X%d" % i])
        P.add("dve", lambda e: e.tensor_tensor(out=xi, in0=xi, in1=b_t, op=ALU.add),
              reads=["X%d" % i, "lnB"], writes=["X%d" % i])

    lnG = {}
    lnB = {}

    for l in range(n_layers):
        lam_init = 0.8 - 0.6 * math.exp(-0.3 * l)
        P.add("sp", lambda e, l=l: e.dma_start(out=bfm_s[:], in_=bfm[l, :, :]), writes=["bfm"], dma="c0")
        P.add("sp", lambda e, l=l: e.dma_start(out=convp_s[:], in_=convp[l, :, :, :]), writes=["convp"], dma="c0")
        P.add("sp", lambda e, l=l: e.dma_start(out=lam_s[:], in_=lamv[l, :, :, :]), writes=["lam"], dma="c0")
        P.add("sp", lambda e, l=l: e.dma_start(out=gsub_s[:], in_=gsub[l, :, :]), writes=["gsub"], dma="c0")
        P.add("sp", lambda e, l=l: e.dma_start(out=br_s[:], in_=b_r[l, :, :]), writes=["br"], dma="c0")
        P.add("sp", lambda e, l=l: e.dma_start(out=wr_s[:], in_=w_r[l].rearrange("(k p) n -> p k n", p=128)),
              writes=["wr"], dma="c0")
        P.add("dve", lambda e: e.tensor_tensor(out=lam_s[:, 0, :], in0=lam_s[:, 0, :], in1=lam_s[:, 1, :], op=ALU.mult),
              reads=["lam"], writes=["lam"])
        P.add("dve", lambda e: e.tensor_tensor(out=lam_s[:, 2, :], in0=lam_s[:, 2, :], in1=lam_s[:, 3, :], op=ALU.mult),
              reads=["lam"], writes=["lam"])
        P.add("dve", lambda e: e.tensor_reduce(out=small[:, 18:19], in_=lam_s[:, 0, :], axis=AX.X, op=ALU.add),
              reads=["lam"], writes=["l1"])
        P.add("dve", lambda e: e.tensor_reduce(out=small[:, 19:20], in_=lam_s[:, 2, :], axis=AX.X, op=ALU.add),
              reads=["lam"], writes=["l2"])
        P.add("act", lambda e: e.activation(out=small[:, 18:20], in_=small[:, 18:20], func=AF.Exp),
              reads=["l1", "l2"], writes=["l1", "l2"])
        P.add("dve", lambda e: e.tensor_tensor(out=small[:, 16:17], in0=small[:, 18:19], in1=small[:, 19:20], op=ALU.subtract),
              reads=["l1", "l2"], writes=["lamv"])
        P.add("dve", lambda e, li=lam_init: e.tensor_scalar(out=small[:, 17:18], in0=small[:, 16:17], scalar1=li, scalar2=-1.0,
                                                          op0=ALU.add, op1=ALU.mult),
              reads=["lamv"], writes=["nlam"])
        P.add("dve", lambda e, li=lam_init: e.tensor_scalar(out=gsub_s[:], in0=gsub_s[:], scalar1=(1.0 - li), scalar2=None,
                                                          op0=ALU.mult),
              reads=["gsub"], writes=["gsub"])

        P.add("sp", lambda e: e.dma_start(out=cosA, in_=cosT[:, :]), writes=["cosS"], dma="cs")
        P.add("sp", lambda e: e.dma_start(out=sinA, in_=sinT[:, :]), writes=["sinS"], dma="sn")
        for i in range(NB):
            for kk in range(2):
                b = bank()
                for j in range(4):
                    k = kk * 4 + j
                    P.add("pe", lambda e, b=b, j=j, i=i, k=k: e.transpose(out=ps[b][:, j * 128:(j + 1) * 128],
                                                                         in_=X[:, i, k * 128:(k + 1) * 128], identity=ident[:]),
                          reads=["X%d" % i, "ident"], writes=["ps%d" % b])
                eng = "dve" if (i + kk) % 2 == 0 else "act"
                if eng == "dve":
                    P.add("dve", lambda e, b=b, i=i, kk=kk: e.tensor_copy(
                        out=xT[:, kk * 4:(kk + 1) * 4, i * 128:(i + 1) * 128],
                        in_=ps[b][:, :].rearrange("p (a t) -> p a t", a=4)),
                        reads=["ps%d" % b], writes=["xT%d" % i])
                else:
                    P.add("act", lambda e, b=b, i=i, kk=kk: e.copy(
                        out=xT[:, kk * 4:(kk + 1) * 4, i * 128:(i + 1) * 128],
                        in_=ps[b][:, :].rearrange("p (a t) -> p a t", a=4)),
                        reads=["ps%d" % b], writes=["xT%d" % i])
        xT_keys = ["xT%d" % i for i in range(NB)]

        def load_w(g, l=l):
            s = g % 3
            P.add("pool", lambda e: e.dma_start(out=Win[s], in_=w_in[l].rearrange("(k p) n -> p k n", p=128)[:, :, g * 256:(g + 1) * 256]),
                  writes=["Win%d" % s], dma="win%d" % s)

        load_w(0)
        load_w(1)
        for g in range(14):
            if g + 2 < 14:
                load_w(g + 2)
            s = g % 3
            W = Win[s]
            wk = "Win%d" % s
            if g < 12:
                for tt in range(4):
                    tsl = slice(tt * 512, (tt + 1) * 512)
                    bA, bB = bank(), bank()
                    for half, b in ((0, bA), (1, bB)):
                        for k in range(8):
                            P.add("pe", lambda e, b=b, k=k, half=half, W=W, tsl=tsl: e.matmul(
                                ps[b][:, :], lhsT=W[:, k, half * 128:(half + 1) * 128], rhs=xT[:, k, tsl],
                                start=(k == 0), stop=(k == 7)),
                                reads=[wk] + xT_keys[tt * 4:(tt + 1) * 4], writes=["ps%d" % b])
                    if g < 4:
                        c = g
                        P.add("act", lambda e, bB=bB, c=c: e.activation(out=scq[0], in_=ps[bB][:, :], func=AF.Sigmoid,
                                                                         bias=bfm_s[:, 2 * c + 1:2 * c + 2]),
                              reads=["ps%d" % bB, "bfm"], writes=["scq0"])
                        P.add("dve", lambda e, bA=bA, c=c, tsl=tsl: e.scalar_tensor_tensor(
                            out=uT[:, c, tsl], in0=ps[bA][:, :], scalar=bfm_s[:, 2 * c:2 * c + 1], in1=scq[0],
                            op0=ALU.add, op1=ALU.mult),
                            reads=["ps%d" % bA, "scq0", "bfm"], writes=["uT%d" % c])
                    else:
                        hh = (g - 4) // 2
                        isk = (g - 4) % 2
                        ch = 8 + 2 * (g - 4)
                        cosS = cosA[:, tsl]
                        sinS = sinA[:, tsl]
                        P.add("dve", lambda e, bA=bA, ch=ch, cosS=cosS: e.scalar_tensor_tensor(
                            out=scq[1], in0=ps[bA][:, :], scalar=bfm_s[:, ch:ch + 1], in1=cosS, op0=ALU.add, op1=ALU.mult) if True else None,
                            reads=["ps%d" % bA, "cosS", "bfm"], writes=["scq1"])
                        P.add("dve", lambda e, bB=bB, ch=ch, sinS=sinS: e.scalar_tensor_tensor(
                            out=scq[2], in0=ps[bB][:, :], scalar=bfm_s[:, ch + 1:ch + 2], in1=sinS, op0=ALU.add, op1=ALU.mult),
                            reads=["ps%d" % bB, "sinS", "bfm"], writes=["scq2"])
                        if isk == 0:
                            P.add("pool", lambda e, hh=hh, tsl=tsl: e.tensor_tensor(out=QT[:, hh, tsl], in0=scq[1], in1=scq[2], op=ALU.add),
                                  reads=["scq1", "scq2"], writes=["QT%d" % hh])
                        else:
                            kst = scq[3].bitcast(BF16)[:, 0:512]
                            P.add("pool", lambda e, kst=kst: e.tensor_tensor(out=kst, in0=scq[1], in1=scq[2], op=ALU.add),
                                  reads=["scq1", "scq2"], writes=["kst"])
                            P.add("sp", lambda e, hh=hh, tsl=tsl, kst=kst: e.dma_start(out=KTb[hh // 2][(hh % 2) * 128:(hh % 2 + 1) * 128, tsl], in_=kst),
                                  reads=["kst"], writes=["KTb%d" % (hh // 2)], dma="kst")
            else:
                vh = g - 12
                for i in range(NB):
                    b = bank()
                    for k in range(8):
                        P.add("pe", lambda e, b=b, k=k, i=i, W=W: e.matmul(
                            ps[b][:, 0:256], lhsT=xT[:, k, i * 128:(i + 1) * 128], rhs=W[:, k, :],
                            start=(k == 0), stop=(k == 7)),
                            reads=[wk, "xT%d" % i], writes=["ps%d" % b])
                    vst = scq[i % 2].bitcast(BF16)[:, 0:256]
                    P.add("dve", lambda e, b=b, vst=vst, vh=vh: e.tensor_tensor(out=vst, in0=ps[b][:, 0:256],
                                                                               in1=tmpA[:, vh * 256:(vh + 1) * 256], op=ALU.add),
                          reads=["ps%d" % b, "bvS"], writes=["scq%d" % (i % 2)])
                    P.add("sp", lambda e, i=i, vh=vh, vst=vst: e.dma_start(out=Vb[vh][i * 128:(i + 1) * 128, :], in_=vst),
                          reads=["scq%d" % (i % 2)], writes=["Vb%d" % vh], dma="vst%d" % (i % 2))
            if g == 3:
                for c in range(4):
                    P.add("sp", lambda e, c=c: e.dma_start(
                        out=Hb[c * 128:(c + 1) * 128, :].rearrange("p (i j) -> p i j", j=32),
                        in_=uT[:, c, :].rearrange("p (i t) -> p i t", t=128)[:, :, 96:128]),
                        reads=["uT%d" % c], writes=["Hb"], dma="hb")
                P.add("pool", lambda e: e.collective_compute("AllGather", ALU.bypass, replica_groups=RG,
                                                             ins=[Hb.ap().opt()], outs=[Hg.ap().opt()]),
                      reads=["Hb"], writes=["Hg"], dma="ccH%d" % l, inc=1)
            if g == 10:
                P.add("sp", lambda e, l=l: e.dma_start(out=tmpA, in_=bv[l, :, :]), writes=["bvS", "lam"], dma="c0")
        P.barrier()
        if stop_after == "B":
            break

        for a in range(2):
            P.add("pool", lambda e, a=a: e.collective_compute("AllGather", ALU.bypass, replica_groups=RG,
                                                              ins=[KTb[a].ap().opt()], outs=[KTg[a].ap().opt()]),
                  reads=["KTb%d" % a], writes=["KTg%d" % a], dma="ccK%d_%d" % (l, a), inc=1)
            P.add("pool", lambda e, a=a: e.collective_compute("AllGather", ALU.bypass, replica_groups=RG,
                                                              ins=[Vb[a].ap().opt()], outs=[Vg[a].ap().opt()]),
                  reads=["Vb%d" % a], writes=["Vg%d" % a], dma="ccV%d_%d" % (l, a), inc=1)

        HG4 = HG.rearrange("p r (i j) -> p r i j", j=32)
        SCQ = ["scq0", "scq1", "scq2", "scq3"]
        extb = [R4[:, 5120:6400].bitcast(BF16).rearrange("p (i c) -> p i c", i=16),
                R4[:, 6400:7680].bitcast(BF16).rearrange("p (i c) -> p i c", i=16)]
        dgs = [R4[:, 0:1984].bitcast(BF16).rearrange("p (k n) -> p k n", k=31),
               SC[:, 0:1984].bitcast(BF16).rearrange("p (k n) -> p k n", k=31)]
        def conv_prep(c):
            q2 = c % 2
            ex_ = extb[q2]
            dg = dgs[q2]
            ek = "extb%d" % q2
            dk = ["dg%d" % q2] + (SCQ if q2 == 1 else ["Win0", "Win1"])
            halo = ex_[:, :, 0:32]
            P.add("sp", lambda e, c=c: e.dma_start(out=HG, in_=Hg.ap().rearrange("(r q) n -> q r n", r=4)[c * 128:(c + 1) * 128, :, :]),
                  reads=["Hg"], writes=["HG"], dma="hg")
            for k in range(31):
                P.add("act", lambda e, c=c, k=k, dg=dg: e.activation(out=dg[:, k, :], in_=identb[:], func=AF.Identity,
                                                                     scale=convp_s[:, c, k:k + 1]),
                      reads=["identb", "convp"], writes=dk)
            P.add("dve", lambda e, ex_=ex_: e.memset(ex_[:, 0, 0:32], 0.0), writes=[ek])
            P.add("dve", lambda e, ex_=ex_: e.tensor_scalar(out=ex_[:, 1:16, 0:32], in0=HG4[:, 3, 0:15, :], scalar1=hcoef[:, 0:1],
                                                            scalar2=None, op0=ALU.mult),
                  reads=["HG", "hcoef"], writes=[ek])
            for q in range(1, 4):
                P.add("dve", lambda e, q=q, halo=halo: e.scalar_tensor_tensor(out=halo, in0=HG4[:, q - 1, :, :], scalar=hcoef[:, q:q + 1],
                                                                              in1=halo, op0=ALU.mult, op1=ALU.add),
                      reads=["HG", "hcoef"], writes=[ek])
            accv = uT[:, c, :].rearrange("p (i t) -> p i t", t=128)
            P.add("act", lambda e, accv=accv, ex_=ex_: e.copy(out=ex_[:, :, 32:160], in_=accv), reads=["uT%d" % c], writes=[ek])

        def conv_compute(c):
            q2 = c % 2
            ex_ = extb[q2]
            dg = dgs[q2]
            ek = "extb%d" % q2
            for tt in range(4):
                b = bank()
                for k in range(31):
                    P.add("pe", lambda e, b=b, k=k, tt=tt, dg=dg, ex_=ex_: e.matmul(
                        ps[b][:, :].rearrange("p (i t) -> p i t", i=4), lhsT=dg[:, k, :],
                        rhs=ex_[:, tt * 4:(tt + 1) * 4, 2 + k:130 + k], start=(k == 0), stop=(k == 30)),
                        reads=[ek, "dg%d" % q2], writes=["ps%d" % b])
                if tt % 2 == 0:
                    P.add("act", lambda e, b=b, c=c, tt=tt: e.activation(out=uT[:, c, tt * 512:(tt + 1) * 512], in_=ps[b][:, :],
                                                                         func=AF.Identity, bias=convp_s[:, c, 31:32]),
                          reads=["ps%d" % b, "convp"], writes=["uT%d" % c])
                else:
                    P.add("dve", lambda e, b=b, c=c, tt=tt: e.tensor_scalar(out=uT[:, c, tt * 512:(tt + 1) * 512], in0=ps[b][:, :],
                                                                            scalar1=convp_s[:, c, 31:32], scalar2=None, op0=ALU.add),
                          reads=["ps%d" % b, "convp"], writes=["uT%d" % c])

        conv_prep(0)
        for c in range(4):
            if c + 1 < 4:
                conv_prep(c + 1)
            conv_compute(c)
        UT = ["uT%d" % c for c in range(4)]
        for tt in range(4):
            tsl = slice(tt * 512, (tt + 1) * 512)
            bm = bank()
            for c in range(4):
                P.add("pe", lambda e, bm=bm, c=c, tsl=tsl: e.matmul(ps[bm][:, :], lhsT=onesf[:], rhs=uT[:, c, tsl],
                                                                     start=(c == 0), stop=(c == 3)),
                      reads=["uT%d" % c, "onesf"], writes=["ps%d" % bm])
            for c in range(4):
                P.add("dve", lambda e, bm=bm, c=c, tsl=tsl: e.tensor_tensor(out=uT[:, c, tsl], in0=uT[:, c, tsl], in1=ps[bm][:, :],
                                                                            op=ALU.subtract),
                      reads=["ps%d" % bm], writes=["uT%d" % c])
            bq = bank()
            for c in range(4):
                P.add("act", lambda e, c=c, tsl=tsl: e.activation(out=scq[c], in_=uT[:, c, tsl], func=AF.Square),
                      reads=["uT%d" % c], writes=["scq%d" % c])
                P.add("pe", lambda e, bq=bq, c=c: e.matmul(ps[bq][:, :], lhsT=onesf[:], rhs=scq[c], start=(c == 0), stop=(c == 3)),
                      reads=["scq%d" % c, "onesf"], writes=["ps%d" % bq])
            P.add("act", lambda e, bq=bq: e.activation(out=scr[0], in_=ps[bq][:, :], func=AF.Ln, bias=epsT[:, 0:1]),
                  reads=["ps%d" % bq, "epsT"], writes=["scr0"])
            P.add("act", lambda e: e.activation(out=scr[0], in_=scr[0], func=AF.Exp, scale=-0.5), reads=["scr0"], writes=["scr0"])
            for c in range(4):
                P.add("dve", lambda e, c=c, tsl=tsl: e.tensor_tensor(out=scq[c], in0=uT[:, c, tsl], in1=scr[0], op=ALU.mult),
                      reads=["uT%d" % c, "scr0"], writes=["scq%d" % c])
                P.add("act", lambda e, c=c, tsl=tsl: e.activation(out=mergedT[:, c, tsl], in_=scq[c], func=AF.Silu,
                                                                  scale=convp_s[:, c, 32:33], bias=convp_s[:, c, 33:34]),
                      reads=["scq%d" % c, "convp"], writes=["mT%d" % c])
        P.barrier()
        if stop_after == "D":
            break

        P.add("pool", lambda e: e.memset(Vh[:, :, 128:129], 1.0), writes=["Vh"])
        P.add("pool", lambda e: e.memset(Qph[64:128, 0, :], 0.0), writes=["Qph"])
        P.add("pool", lambda e: e.memset(Qph[0:64, 1, :], 0.0), writes=["Qph"])
        for h in range(4):
            s = h % 2
            P.add("sp", lambda e, h=h, s=s: e.dma_start(out=KTh[s], in_=KTg[h // 2].ap().rearrange("(r q) t -> q r t", r=4)[(h % 2) * 128:(h % 2 + 1) * 128, :, :]),
                  reads=["KTg%d" % (h // 2)], writes=["KTh%d" % s], dma="kth%d" % s)
            P.add("sp", lambda e, h=h: e.dma_start(out=Vh[:, :, 0:128], in_=Vg[h // 2].ap().rearrange("(j p) e -> p j e", p=128)[:, :, (h % 2) * 128:(h % 2 + 1) * 128]),
                  reads=["Vg%d" % (h // 2)], writes=["Vh"], dma="vh")
            P.add("pool", lambda e, h=h: e.tensor_copy(out=Qph[0:64, 0, :], in_=QT[0:64, h, :]), reads=["QT%d" % h], writes=["Qph"])
            P.add("act", lambda e, h=h: e.copy(out=Qph[64:128, 1, :], in_=QT[64:128, h, :]), reads=["QT%d" % h], writes=["Qph"])
            if l == 0 and h == 0:
                P.add("pool", lambda e: e.memset(zt[:, :], 0.0), writes=["zt"])
                P.add("pool", lambda e: e.dma_start(
                    out=xs[0:NE * CAP, :].rearrange("(n p) d -> p n d", p=128),
                    in_=zt[:, :].rearrange("p (o d) -> p o d", o=1).to_broadcast([128, NE * CAP // 128, D])),
                    reads=["zt"], writes=["xs"], dma="z0")
                P.add("pool", lambda e: e.dma_start(out=xs[NE * CAP:NE * CAP + 1, :], in_=zt[0:1, :]), reads=["zt"], writes=["xs"], dma="z0")
            tiles = [(i, pp) for i in range(NB) for pp in range(2 * i + 2)]
            LA = 3

            def qk(n, h=h, s=s):
                i, pp = tiles[n]
                b = n % 3
                for kbp in range(2):
                    jb = 2 * pp + kbp
                    rr, il = jb % 4, jb // 4
                    masked = jb >= 4 * i
                    P.add("pe", lambda e, b=b, kbp=kbp, rr=rr, il=il, i=i, masked=masked: e.matmul(
                        ps[b][:, kbp * 256:(kbp + 1) * 256].rearrange("p (m q) -> p m q", m=2),
                        lhsT=KTh[s][:, rr, il * 128:(il + 1) * 128], rhs=Qph[:, :, i * 128:(i + 1) * 128],
                        start=True, stop=(not masked)),
                        reads=["KTh%d" % s, "Qph"], writes=["ps%d" % b])
                    if masked:
                        for m in range(2):
                            osl = slice((kbp * 2 + m) * 128, (kbp * 2 + m + 1) * 128)
                            P.add("pe", lambda e, b=b, osl=osl, q=jb - 4 * i, m=m: e.matmul(
                                ps[b][:, osl], lhsT=identb[:], rhs=maskb[:, q, :], start=False, stop=(m == 1)),
                                reads=["identb", "maskb"], writes=["ps%d" % b])
                P.add("act", lambda e, b=b, n=n: e.activation(out=PT[:, n % 4, :], in_=ps[b][:, :], func=AF.Exp, scale=0.125),
                      reads=["ps%d" % b], writes=["PT%d" % (n % 4)])

            def pv(n, h=h):
                i, pp = tiles[n]
                ob = 4 + i % 2
                for kbp in range(2):
                    jb = 2 * pp + kbp
                    rr, il = jb % 4, jb // 4
                    for m in range(2):
                        osl = slice((kbp * 2 + m) * 128, (kbp * 2 + m + 1) * 128)
                        P.add("pe", lambda e, ob=ob, m=m, n=n, osl=osl, rr=rr, il=il, first=(pp == 0 and kbp == 0),
                              last=(pp == 2 * i + 1 and kbp == 1): e.matmul(
                            ps[ob + 2 * m][:, 0:129], lhsT=PT[:, n % 4, osl], rhs=Vh[:, rr * 16 + il, :],
                            start=first, stop=last),
                            reads=["PT%d" % (n % 4), "Vh"], writes=["ps%d" % (ob + 2 * m)])
                if pp == 2 * i + 1:
                    pending.append(epilogue_stages(i, h))

            def epilogue_stages(i, h=h):
                ob = 4 + i % 2
                O = ps[ob]
                OB = ps[ob + 2]
                okey = "ps%d" % ob
                okeyB = "ps%d" % (ob + 2)
                tb = 3
                sA = scr[i % 2]
                sk = "scr%d" % (i % 2)
                sm = small[:, 32 + 8 * (i % 2):40 + 8 * (i % 2)]
                mk = "sm%d" % (i % 2)
                onb = sA[:, 384:448].bitcast(BF16)

                def st1():
                    P.add("dve", lambda e: e.reciprocal(out=sm[:, 0:1], in_=O[:, 128:129]), reads=[okey], writes=[mk])
                    P.add("dve", lambda e: e.reciprocal(out=sm[:, 1:2], in_=OB[:, 128:129]), reads=[okeyB, mk], writes=[mk])
                    P.add("dve", lambda e: e.tensor_tensor(out=sm[:, 2:3], in0=sm[:, 1:2], in1=small[:, 17:18], op=ALU.mult),
                          reads=[mk, "nlam"], writes=[mk])
                    P.add("dve", lambda e: e.tensor_scalar(out=sA[:, 0:128], in0=O[:, 0:128], scalar1=sm[:, 0:1],
                                                           scalar2=None, op0=ALU.mult),
                          reads=[okey, mk], writes=[sk])
                    P.add("dve", lambda e: e.scalar_tensor_tensor(
                        out=sA[:, 128:256], in0=OB[:, 0:128], scalar=sm[:, 2:3], in1=sA[:, 0:128], op0=ALU.mult, op1=ALU.add),
                        reads=[okeyB, mk, sk], writes=[sk])
                    P.add("dve", lambda e: e.memset(sm[:, 3:4], 0.0), writes=[mk])
                    P.add("dve", lambda e: e.scalar_tensor_tensor(
                        out=sA[:, 256:384], in0=sA[:, 128:256], scalar=1.0, in1=sA[:, 128:256], op0=ALU.mult, op1=ALU.mult,
                        accum_out=sm[:, 3:4]),
                        reads=[sk, mk], writes=[sk, mk])

                def st2():
                    P.add("act", lambda e: e.activation(out=sm[:, 4:5], in_=sm[:, 3:4], func=AF.Ln, scale=1.0 / 128.0,
                                                        bias=epsT[:, 0:1]),
                          reads=[mk, "epsT"], writes=[mk])
                    P.add("act", lambda e: e.activation(out=sm[:, 5:6], in_=sm[:, 4:5], func=AF.Exp, scale=-0.5),
                          reads=[mk], writes=[mk])

                def st3():
                    P.add("dve", lambda e: e.scalar_tensor_tensor(
                        out=onb, in0=sA[:, 128:256], scalar=sm[:, 5:6], in1=gsub_s[:], op0=ALU.mult, op1=ALU.mult),
                        reads=[sk, mk, "gsub"], writes=[sk])
                    P.add("pe", lambda e: e.transpose(out=ps[tb][:, :].bitcast(BF16)[:, 0:128], in_=onb, identity=identb[:]),
                          reads=[sk, "identb"], writes=["ps%d" % tb])

                def st4():
                    P.add("dve", lambda e: e.tensor_copy(out=mergedT[:, 4 + h, i * 128:(i + 1) * 128],
                                                         in_=ps[tb][:, :].bitcast(BF16)[:, 0:128]),
                          reads=["ps%d" % tb], writes=["mT%d" % (4 + h)])
                return [st1, (lambda: None), (lambda: None), (lambda: None), st2, (lambda: None), (lambda: None), (lambda: None), st3, (lambda: None), (lambda: None), st4]

            pending = []

            def advance():
                for stg in list(pending):
                    stg.pop(0)()
                    if not stg:
                        pending.remove(stg)

            for n in range(len(tiles) + LA):
                if n < len(tiles):
                    qk(n)
                advance()
                if n >= LA:
                    pv(n - LA)
            while pending:
                advance()
        P.barrier()
        if stop_after == "E":
            break

        woS = R2[:, :].bitcast(BF16)[:, 0:8192].rearrange("p (k n) -> p k n", k=8)
        lnT = R2[:, 4096:8192].rearrange("p (w d) -> p w d", w=4)
        lnG[0], lnB[0], lnG[1], lnB[1] = lnT[:, 0, :], lnT[:, 1, :], lnT[:, 2, :], lnT[:, 3, :]
        P.add("pool", lambda e, l=l: e.dma_start(out=woS, in_=w_out[l].rearrange("(k p) n -> p k n", p=128)), writes=["woS"], dma="wo")
        P.add("sp", lambda e, l=l: e.dma_start(out=lnT, in_=lnp[l].rearrange("w p d -> p w d")), writes=["lnG", "lnB"], dma="ln")
        x1b = [R4b[:, s * 1024:(s + 1) * 1024] for s in range(6)]
        x1T = R4[:, 1024:2048].rearrange("p (k t) -> p k t", k=8)
        rt = R4[:, 4096:4352]
        MT = ["mT%d" % k for k in range(8)]
        x1Ts = [R4[:, 3072:4096].rearrange("p (k t) -> p k t", k=8), R4[:, 4096:5120].rearrange("p (k t) -> p k t", k=8)]
        rts = [R4[:, 5120 + 256 * q:5376 + 256 * q] for q in range(6)]
        def pass1_block(i):
            bsl = slice(i * 128, (i + 1) * 128)
            xk = "X%d" % i
            for nt in range(2):
                b = bank()
                for k in range(8):
                    P.add("pe", lambda e, b=b, k=k, bsl=bsl, nt=nt: e.matmul(ps[b][:, :], lhsT=mergedT[:, k, bsl],
                                                                             rhs=woS[:, k, nt * 512:(nt + 1) * 512],
                                                                             start=(k == 0), stop=(k == 7)),
                          reads=["mT%d" % k, "woS"], writes=["ps%d" % b])
                P.add("dve", lambda e, b=b, i=i, nt=nt: e.scalar_tensor_tensor(
                    out=X[:, i, nt * 512:(nt + 1) * 512], in0=X[:, i, nt * 512:(nt + 1) * 512], scalar=ALPHA, in1=ps[b][:, :],
                    op0=ALU.mult, op1=ALU.add),
                    reads=["ps%d" % b], writes=[xk])
            ln_block(l, i, 0)

        p1 = []
        for i in range(NB):
            P.begin_record()
            pass1_block(i)
            p1.append(P.end_record())
        for i in range(0, NB, 2):
            P.replay_interleaved([p1[i], p1[i + 1]])

        P.barrier()
        R1w = R1[:, :]
        R2w = R2[:, :].bitcast(BF16)
        def _w8(ap):
            return ap.rearrange("p (k n) -> p k n", k=8)
        def _w4(ap):
            return ap.rearrange("p (k n) -> p k n", k=4)
        Wg = [_w8(R1w[:, 0:4096]), _w8(R1w[:, 12288:16384]), _w8(R2w[:, 0:4096])]
        Wu = [_w8(R1w[:, 4096:8192]), _w8(R3[:, 0:4096]), _w8(R2w[:, 4096:8192])]
        Wd = [_w4(R1w[:, 8192:12288]), _w4(R3[:, 4096:8192]), _w4(R2w[:, 8192:12288])]

        def load_expert_w(ex, l=l):
            s = ex % 3
            P.add("pool", lambda e: e.dma_start(out=Wg[s], in_=w_g[l, ex].rearrange("(k p) n -> p k n", p=128)),
                  writes=["Wg%d" % s], dma="wg%d" % s)
            P.add("pool", lambda e: e.dma_start(out=Wu[s], in_=w_u[l, ex].rearrange("(k p) n -> p k n", p=128)),
                  writes=["Wu%d" % s], dma="wu%d" % s)
            P.add("pool", lambda e: e.dma_start(out=Wd[s], in_=w_d[l, ex].rearrange("(k p) n -> p k n", p=128)),
                  writes=["Wd%d" % s], dma="wd%d" % s)

        if ne_decl == NE:
            for ex in range(3):
                load_expert_w(ex)

        def route_block(i, part):
            par = i % 2
            xk = "X%d" % i
            x1T = x1Ts[par]
            x1Tk = "x1T%d" % par
            rt = rts[i % 6]
            xb = x1b[i % 6]
            xbk = "x1b%d" % (i % 6)
            RT = ["rt%d" % (i % 6)]
            if part == 1:
                return route_chain(i, rt, RT, xb, xbk)
            P.add("act", lambda e: e.copy(out=xb, in_=X[:, i, :]), reads=[xk], writes=[xbk])
            for kk in range(2):
                b = bank()
                for j in range(4):
                    k = kk * 4 + j
                    P.add("pe", lambda e, b=b, j=j, k=k: e.transpose(out=ps[b][:, j * 128:(j + 1) * 128],
                                                                   in_=X[:, i, k * 128:(k + 1) * 128], identity=ident[:]),
                          reads=[xk, "ident"], writes=["ps%d" % b])
                if kk == 0:
                    P.add("dve", lambda e, b=b, kk=kk: e.tensor_copy(out=x1T[:, kk * 4:(kk + 1) * 4, :],
                                                                    in_=ps[b][:, :].rearrange("p (a t) -> p a t", a=4)),
                          reads=["ps%d" % b], writes=[x1Tk])
                else:
                    P.add("act", lambda e, b=b, kk=kk: e.copy(out=x1T[:, kk * 4:(kk + 1) * 4, :],
                                                             in_=ps[b][:, :].rearrange("p (a t) -> p a t", a=4)),
                          reads=["ps%d" % b], writes=[x1Tk])
            bl = bank()
            for k in range(8):
                P.add("pe", lambda e, bl=bl, k=k: e.matmul(ps[bl][:, 0:36], lhsT=x1T[:, k, :], rhs=wr_s[:, k, :],
                                                          start=(k == 0), stop=(k == 7)),
                      reads=[x1Tk, "wr"], writes=["ps%d" % bl])
            L = rt[:, 0:36]

            def dv(fn, extra_r=(), extra_w=()):
                P.add("dve", fn, reads=RT + list(extra_r), writes=RT + list(extra_w))

            P.add("dve", lambda e, bl=bl: e.tensor_tensor(out=rt[:, 0:36], in0=ps[bl][:, 0:36], in1=br_s[:], op=ALU.add),
                  reads=["ps%d" % bl, "br"], writes=RT)

        def route_chain(i, rt, RT, xb, xbk):
            def dv(fn, extra_r=(), extra_w=()):
                P.add("dve", fn, reads=RT + list(extra_r), writes=RT + list(extra_w))

            dv(lambda e: e.tensor_reduce(out=rt[:, 40:41], in_=rt[:, 0:4], axis=AX.X, op=ALU.max))
            dv(lambda e: e.tensor_tensor(out=rt[:, 44:48], in0=rt[:, 0:4], in1=rt[:, 40:41].to_broadcast([128, 4]), op=ALU.is_equal))
            dv(lambda e: e.tensor_scalar(out=rt[:, 41:42], in0=rt[:, 40:41], scalar1=-1.0, scalar2=None, op0=ALU.mult))
            dv(lambda e: e.memset(rt[:, 42:43], 0.0))
            P.add("act", lambda e: e.activation(out=rt[:, 48:52], in_=rt[:, 0:4], func=AF.Exp, bias=rt[:, 41:42], accum_out=rt[:, 42:43]),
                  reads=RT, writes=RT)
            dv(lambda e: e.reciprocal(out=rt[:, 43:44], in_=rt[:, 42:43]))
            dv(lambda e: e.tensor_scalar(out=rt[:, 52:56], in0=rt[:, 44:48], scalar1=-1.0, scalar2=1e9, op0=ALU.add, op1=ALU.mult))
            for g in range(4):
                dv(lambda e, g=g: e.tensor_scalar(out=rt[:, 64 + 8 * g:72 + 8 * g], in0=rt[:, 4 + 8 * g:12 + 8 * g],
                                                  scalar1=rt[:, 52 + g:53 + g], scalar2=None, op0=ALU.add))
            dv(lambda e: e.max(out=rt[:, 96:104], in_=rt[:, 64:96]))
            dv(lambda e: e.tensor_tensor(out=rt[:, 104:136], in0=rt[:, 64:96], in1=rt[:, 96:97].to_broadcast([128, 32]), op=ALU.is_equal))
            dv(lambda e: e.tensor_tensor(out=rt[:, 136:168], in0=rt[:, 64:96], in1=rt[:, 97:98].to_broadcast([128, 32]), op=ALU.is_ge))
            dv(lambda e, i=i: e.tensor_copy(out=selB[:, i, :], in_=rt[:, 136:168]), extra_w=["selB%d" % i])
            dv(lambda e: e.tensor_tensor(out=rt[:, 168:200], in0=rt[:, 136:168], in1=rt[:, 104:136], op=ALU.subtract))
            dv(lambda e: e.tensor_tensor(out=rt[:, 56:57], in0=rt[:, 97:98], in1=rt[:, 96:97], op=ALU.subtract))
            P.add("act", lambda e: e.activation(out=rt[:, 57:58], in_=rt[:, 56:57], func=AF.Exp), reads=RT, writes=RT)
            dv(lambda e: e.tensor_scalar(out=rt[:, 58:59], in0=rt[:, 57:58], scalar1=1.0, scalar2=None, op0=ALU.add))
            dv(lambda e: e.reciprocal(out=rt[:, 58:59], in_=rt[:, 58:59]))
            dv(lambda e: e.tensor_tensor(out=rt[:, 59:60], in0=rt[:, 58:59], in1=rt[:, 43:44], op=ALU.mult))
            dv(lambda e: e.tensor_tensor(out=rt[:, 60:61], in0=rt[:, 59:60], in1=rt[:, 57:58], op=ALU.mult))
            bc = bank()
            for ip in range(i + 1):
                P.add("pe", lambda e, bc=bc, ip=ip, i=i: e.matmul(ps[bc][:, 0:32], lhsT=(trib[:] if ip == i else onesb[:]),
                                                                  rhs=selB[:, ip, :], start=(ip == 0), stop=(ip == i)),
                      reads=["selB%d" % ip, "trib", "onesb"], writes=["ps%d" % bc])
            P.add("dve", lambda e, bc=bc: e.tensor_tensor(out=rt[:, 200:232], in0=ps[bc][:, 0:32], in1=ecs[:], op=ALU.add),
                  reads=["ps%d" % bc, "ecs"] + RT, writes=RT)
            P.add("dve", lambda e, bc=bc: e.tensor_scalar(out=rt[:, 4:36], in0=ps[bc][:, 0:32], scalar1=float(CAP), scalar2=None, op0=ALU.is_lt),
                  reads=["ps%d" % bc] + RT, writes=RT)
            dv(lambda e: e.tensor_scalar(out=rt[:, 200:232], in0=rt[:, 200:232], scalar1=-BIG, scalar2=None, op0=ALU.add))
            dv(lambda e: e.tensor_tensor(out=rt[:, 200:232], in0=rt[:, 200:232], in1=rt[:, 4:36], op=ALU.mult))
            dv(lambda e: e.tensor_scalar(out=rt[:, 200:232], in0=rt[:, 200:232], scalar1=BIG, scalar2=None, op0=ALU.add))
            for kx, so in ((0, 104), (1, 168)):
                dv(lambda e, so=so: e.tensor_tensor(out=rt[:, 64:96], in0=rt[:, so:so + 32], in1=rt[:, 200:232], op=ALU.mult))
                dv(lambda e, kx=kx: e.tensor_reduce(out=rt[:, 61 + kx:62 + kx], in_=rt[:, 64:96], axis=AX.X, op=ALU.add))
                dv(lambda e, kx=kx, i=i: e.tensor_copy(out=destI[:, i, kx:kx + 1], in_=rt[:, 61 + kx:62 + kx]), extra_w=["dest%d" % i])
                dv(lambda e, kx=kx: e.tensor_scalar(out=rt[:, 36 + kx:37 + kx], in0=rt[:, 61 + kx:62 + kx], scalar1=BIG - 0.5, scalar2=None,
                                                    op0=ALU.is_lt))
                dv(lambda e, kx=kx: e.tensor_scalar(out=rt[:, 38:39], in0=rt[:, 61 + kx:62 + kx], scalar1=2.0, scalar2=None, op0=ALU.mult))
                dv(lambda e, kx=kx: e.tensor_scalar(out=rt[:, 39:40], in0=rt[:, 61 + kx:62 + kx], scalar1=2.0, scalar2=1.0, op0=ALU.mult, op1=ALU.add))
                dv(lambda e, kx=kx, i=i: e.tensor_copy(out=destG[:, i, 2 * kx:2 * kx + 2], in_=rt[:, 38:40]), extra_w=["dest%d" % i])
                dv(lambda e, kx=kx, i=i: e.tensor_tensor(out=wgt[:, i, kx:kx + 1], in0=rt[:, 59 + kx:60 + kx], in1=rt[:, 36 + kx:37 + kx],
                                                         op=ALU.mult), extra_w=["wgt%d" % i])
            for kx in range(2):
                P.add("pool", lambda e, xb=xb, i=i, kx=kx: e.indirect_dma_start(
                    out=xs[:, :], out_offset=bass.IndirectOffsetOnAxis(ap=destI[:, i, kx:kx + 1], axis=0),
                    in_=xb, in_offset=None, bounds_check=None, oob_is_err=False),
                    reads=[xbk, "dest%d" % i], writes=["xs"], dma="sc%d" % (i % 6))

        pre, chn = [], []
        for i in range(NB):
            P.begin_record()
            route_block(i, 0)
            pre.append(P.end_record())
            P.begin_record()
            route_block(i, 1)
            chn.append(P.end_record())
        P.replay_interleaved([pre[0], pre[1]])
        for i in range(0, NB, 2):
            if i + 2 < NB:
                P.replay_interleaved([pre[i + 2], pre[i + 3]])
            P.replay_interleaved([chn[i], chn[i + 1]])
        P.barrier()
        if stop_after == "F":
            break

        xg = [R4b[:, 8704 + s * 2048:8704 + (s + 1) * 2048].rearrange("p (b d) -> p b d", b=2) for s in range(2)]
        xgT = R4b[:, 12800:14848].rearrange("p (k t) -> p k t", k=8)
        actT = R4b[:, 14848:15872].rearrange("p (k t) -> p k t", k=4)
        ysb = [R4[:, s * 1024:(s + 1) * 1024] for s in range(2)]
        def load_xg(ex):
            q = ex % 2
            P.add("sp", lambda e: e.dma_start(out=xg[q], in_=xs[ex * CAP:(ex + 1) * CAP, :].rearrange("(b p) d -> p b d", p=128)),
                  reads=["xs"], writes=["xg%d" % q], dma="xg%d" % q)

        for ex in range(NE):
            s = ex % 3
            xs_ = ex % 2
            if ex >= 3:
                load_expert_w(ex)
            if ex == 0:
                load_xg(0)
            if ex + 1 < NE:
                load_xg(ex + 1)
            for sbk in range(2):
                for kk in range(2):
                    b = bank()
                    for j in range(4):
                        k = kk * 4 + j
                        P.add("pe", lambda e, b=b, j=j, k=k, xs_=xs_, sbk=sbk: e.transpose(
                            out=ps[b][:, :].bitcast(BF16)[:, j * 128:(j + 1) * 128], in_=xg[xs_][:, sbk, k * 128:(k + 1) * 128], identity=identb[:]),
                            reads=["xg%d" % xs_, "identb"], writes=["ps%d" % b])
                    if (sbk + kk) % 2 == 0:
                        P.add("dve", lambda e, b=b, kk=kk, sbk=sbk: e.tensor_copy(
                            out=xgT[:, kk * 4:(kk + 1) * 4, sbk * 128:(sbk + 1) * 128],
                            in_=ps[b][:, :].bitcast(BF16)[:, 0:512].rearrange("p (a t) -> p a t", a=4)),
                            reads=["ps%d" % b], writes=["xgT"])
                    else:
                        P.add("act", lambda e, b=b, kk=kk, sbk=sbk: e.copy(
                            out=xgT[:, kk * 4:(kk + 1) * 4, sbk * 128:(sbk + 1) * 128],
                            in_=ps[b][:, :].bitcast(BF16)[:, 0:512].rearrange("p (a t) -> p a t", a=4)),
                            reads=["ps%d" % b], writes=["xgT"])
            for hc in range(4):
                b = bank()
                for which, Wm, wkey in ((0, Wg, "Wg%d" % s), (1, Wu, "Wu%d" % s)):
                    for k in range(8):
                        P.add("pe", lambda e, b=b, k=k, hc=hc, which=which, Wm=Wm, s=s: e.matmul(
                            ps[b][:, which * 256:(which + 1) * 256], lhsT=Wm[s][:, k, hc * 128:(hc + 1) * 128], rhs=xgT[:, k, :],
                            start=(k == 0), stop=(k == 7)),
                            reads=[wkey, "xgT"], writes=["ps%d" % b])
                sq = scq[hc % 2]
                P.add("act", lambda e, b=b, sq=sq: e.activation(out=sq[:, 0:256], in_=ps[b][:, 0:256], func=AF.Silu),
                      reads=["ps%d" % b], writes=["scq%d" % (hc % 2)])
                P.add("dve", lambda e, b=b, sq=sq, hc=hc: e.tensor_tensor(out=actT[:, hc, :], in0=sq[:, 0:256], in1=ps[b][:, 256:512], op=ALU.mult),
                      reads=["ps%d" % b, "scq%d" % (hc % 2)], writes=["actT"])
            for sbk in range(2):
                for nt in range(2):
                    b = bank()
                    for hc in range(4):
                        P.add("pe", lambda e, b=b, hc=hc, sbk=sbk, nt=nt, s=s: e.matmul(
                            ps[b][:, :], lhsT=actT[:, hc, sbk * 128:(sbk + 1) * 128], rhs=Wd[s][:, hc, nt * 512:(nt + 1) * 512],
                            start=(hc == 0), stop=(hc == 3)),
                            reads=["actT", "Wd%d" % s], writes=["ps%d" % b])
                    if nt == 0:
                        P.add("act", lambda e, b=b, sbk=sbk: e.copy(out=ysb[sbk][:, 0:512], in_=ps[b][:, :]),
                              reads=["ps%d" % b], writes=["ysb%d" % sbk])
                    else:
                        P.add("dve", lambda e, b=b, sbk=sbk: e.tensor_copy(out=ysb[sbk][:, 512:1024], in_=ps[b][:, :]),
                              reads=["ps%d" % b], writes=["ysb%d" % sbk])
                P.add("sp", lambda e, ex=ex, sbk=sbk: e.dma_start(out=ys[(ex * CAP + sbk * 128) * 2:(ex * CAP + (sbk + 1) * 128) * 2, :].rearrange("(p h) c -> p (h c)", h=2), in_=ysb[sbk]),
                      reads=["ysb%d" % sbk], writes=["ys"], dma="ys%d" % sbk)

        P.barrier()
        R1f = R1[:, :].bitcast(F32)
        Ybuf = [[R1f[:, (2 * q + kx) * 1024:(2 * q + kx + 1) * 1024] for kx in range(2)] for q in range(2)]

        def combine_block(i):
            xk = "X%d" % i
            q = i % 2
            y12 = Ybuf[q]
            for kx in range(2):
                for hf in range(2):
                    P.add("pool", lambda e, kx=kx, hf=hf: e.indirect_dma_start(
                        out=y12[kx][:, hf * 512:(hf + 1) * 512], out_offset=None, in_=ys[:, :],
                        in_offset=bass.IndirectOffsetOnAxis(ap=destG[:, i, 2 * kx + hf:2 * kx + hf + 1], axis=0),
                        bounds_check=None, oob_is_err=False),
                        reads=["ys", "dest%d" % i], writes=["y%d_%d_%d" % (q, kx, hf)], dma="ga%d_%d_%d" % (q, kx, hf))
            ya = ["y%d_0_0" % q, "y%d_0_1" % q]
            yb = ["y%d_1_0" % q, "y%d_1_1" % q]
            P.add("dve", lambda e: e.tensor_scalar(out=y12[0], in0=y12[0], scalar1=wgt[:, i, 0:1], scalar2=None, op0=ALU.mult),
                  reads=["wgt%d" % i], writes=ya)
            P.add("dve", lambda e: e.scalar_tensor_tensor(out=y12[0], in0=y12[1], scalar=wgt[:, i, 1:2], in1=y12[0],
                                                          op0=ALU.mult, op1=ALU.add),
                  reads=["wgt%d" % i] + yb, writes=ya)
            P.add("dve", lambda e: e.scalar_tensor_tensor(out=X[:, i, :], in0=X[:, i, :], scalar=ALPHA, in1=y12[0],
                                                          op0=ALU.mult, op1=ALU.add),
                  reads=ya, writes=[xk])
            ln_block(l, i, 1)
            if l == n_layers - 1:
                P.add("sp", lambda e: e.dma_start(out=out[i, :, :], in_=X[:, i, :]), reads=[xk], writes=["out"], dma="out%d" % (i % 2))

        hrec = []
        for i in range(NB):
            P.begin_record()
            combine_block(i)
            hrec.append(P.end_record())
        for i in range(0, NB, 2):
            P.replay_interleaved([hrec[i], hrec[i + 1]])
        P.barrier()

    if dbg is not None:
        loc = locals()
        for (dn, dfn, dshape, ddt) in dbg:
            P.add("sp", lambda e, dn=dn, dfn=dfn: e.dma_start(out=dbg_out[dn].ap(), in_=dfn(loc)), writes=["dbgout"], dma="dbg")
    P.barrier()
    with nc.Block() as block:
        P.emit(nc, block, es)
    es.close()
    return nc


RG = [[0, 1, 2, 3], [4, 5, 6, 7]]

def _col_order():
    cols = []
    for c in range(4):
        cols += list(range(c * 128, (c + 1) * 128))
        cols += list(range(512 + c * 128, 512 + (c + 1) * 128))
    perm = np.array([(p // 64) * 64 + ((p % 64) + 32) % 64 for p in range(128)])
    for h in range(4):
        for off in (1024, 1536):
            base = off + h * 128
            cols += list(base + np.arange(128))
            cols += list(base + perm)
    cols += list(range(2048, 2560))
    return np.array(cols, dtype=np.int64)


_COLS = _col_order()


def _host_inputs(inp):
    f32 = np.float32
    x = np.asarray(inp["x"], f32)
    w_in = np.asarray(inp["w_in"], f32)[:, :, _COLS]
    b_in = np.asarray(inp["b_in"], f32)[:, _COLS]
    shared = {}
    shared["w_in"] = np.ascontiguousarray(w_in)
    shared["bfm"] = np.ascontiguousarray(b_in[:, :3072].reshape(DEPTH, 24, 128).transpose(0, 2, 1))
    shared["bv"] = np.ascontiguousarray(np.broadcast_to(b_in[:, None, 3072:], (DEPTH, 128, 512)))
    cw = np.asarray(inp["conv_w"], f32)[:, :, 0, :]
    convp = np.zeros((DEPTH, 128, 4, 34), f32)
    convp[:, :, :, 0:31] = cw.reshape(DEPTH, 31, 4, 128).transpose(0, 3, 2, 1)
    convp[:, :, :, 31] = np.asarray(inp["conv_b"], f32).reshape(DEPTH, 4, 128).transpose(0, 2, 1)
    convp[:, :, :, 32] = np.asarray(inp["conv_ln_g"], f32).reshape(DEPTH, 4, 128).transpose(0, 2, 1)
    convp[:, :, :, 33] = np.asarray(inp["conv_ln_b"], f32).reshape(DEPTH, 4, 128).transpose(0, 2, 1)
    shared["convp"] = convp
    lam = np.stack([np.asarray(inp[k], f32) for k in ("lam_q1", "lam_k1", "lam_q2", "lam_k2")], axis=1)
    shared["lamv"] = np.ascontiguousarray(np.broadcast_to(lam[:, None], (DEPTH, 128, 4, 64)))
    shared["gsub"] = np.ascontiguousarray(np.broadcast_to(np.asarray(inp["subln_g"], f32)[:, None, :], (DEPTH, 128, 128)))
    shared["w_out"] = np.ascontiguousarray(np.asarray(inp["w_out"], f32))
    lnp = np.stack([np.asarray(inp[k], f32) for k in ("ln1_g", "ln1_b", "ln2_g", "ln2_b")], axis=1)
    shared["lnp"] = np.ascontiguousarray(np.broadcast_to(lnp[:, :, None, :], (DEPTH, 4, 128, D)))
    shared["w_r"] = np.ascontiguousarray(np.concatenate([np.asarray(inp["w_rg"], f32), np.asarray(inp["w_re"], f32)], axis=-1))
    b_r = np.concatenate([np.asarray(inp["b_rg"], f32), np.asarray(inp["b_re"], f32)], axis=-1)
    shared["b_r"] = np.ascontiguousarray(np.broadcast_to(b_r[:, None, :], (DEPTH, 128, 36)))
    shared["w_gate_e"] = np.ascontiguousarray(np.asarray(inp["w_gate_e"], f32))
    shared["w_up_e"] = np.ascontiguousarray(np.asarray(inp["w_up_e"], f32))
    shared["w_down_e"] = np.ascontiguousarray(np.asarray(inp["w_down_e"], f32))
    shared["ident"] = np.eye(128, dtype=f32)
    shared["tri"] = np.triu(np.ones((128, 128), f32), k=1)
    shared["ec"] = np.ascontiguousarray(np.broadcast_to((np.arange(NE, dtype=f32) * CAP)[None, :], (128, NE)))
    half = 32
    inv_freq = (1.0 / (np.float32(10000.0) ** (np.arange(half, dtype=f32) * f32(2.0) / f32(64)))).astype(f32)
    maps = []
    for c in range(8):
        b, r = c // 4, c % 4
        m = dict(shared)
        blocks = np.arange(NB) * 4 + r
        m["x"] = np.ascontiguousarray(x[b].reshape(64, 128, D)[blocks])
        pos = (blocks[:, None] * 128 + np.arange(128)[None, :]).reshape(-1).astype(f32)
        ang = (pos[:, None] * inv_freq[None, :]).astype(f32)
        cos, sin = np.cos(ang).astype(f32), np.sin(ang).astype(f32)
        cos64 = np.concatenate([cos, cos], axis=1).T
        sin64 = np.concatenate([-sin, sin], axis=1).T
        m["cosT"] = np.ascontiguousarray(np.concatenate([cos64, cos64], axis=0))
        m["sinT"] = np.ascontiguousarray(np.concatenate([sin64, sin64], axis=0))
        mb = np.zeros((128, 4, 128), f32)
        for q in range(4):
            if q > r:
                mb[:, q, :] = NEG
            elif q == r:
                mb[64:, q, :64] = NEG
        m["maskb"] = mb
        hc = np.zeros((128, 4), f32)
        hc[:, r] = 1.0
        m["hcoef"] = hc
        maps.append(m)
    return maps


_NC_CACHE = {}


def kernel(**inputs):
    maps = _host_inputs(inputs)
    if "nc" not in _NC_CACHE:
        _NC_CACHE["nc"] = build()
    nc = _NC_CACHE["nc"]
    res = run_bass_kernel_spmd(nc, maps, core_ids=list(range(8)))
    outp = np.zeros((2, 64, 128, D), np.float32)
    for c in range(8):
        b, r = c // 4, c % 4
        outp[b, np.arange(NB) * 4 + r] = np.asarray(res.results[c]["out"], np.float32)
    return outp.reshape(2, 8192, D)
```

```python
import math
from contextlib import ExitStack

import numpy as np
import concourse.bass as bass
import concourse.mybir as mybir
from concourse.bass_utils import run_bass_kernel_spmd

F32 = mybir.dt.float32
BF16 = mybir.dt.bfloat16
I32 = mybir.dt.int32
ALU = mybir.AluOpType
AF = mybir.ActivationFunctionType
AX = mybir.AxisListType

D = 1024
NB = 16
T = NB * 128
DEPTH = 2
CAP = 256
NE = 32
ALPHA = (2.0 * DEPTH) ** 0.25
LN_EPS = 1e-5
BIG = float(NE * CAP)
NEG = -30000.0

ENGS = ("pe", "act", "dve", "pool", "sp")


class Op:
    __slots__ = ("eng", "fn", "deps", "dma", "signal", "tok", "inc")

    def __init__(self, eng, fn, dma, inc):
        self.eng, self.fn, self.dma, self.inc = eng, fn, dma, inc
        self.deps = []
        self.signal = False
        self.tok = None


class Prog:
    def __init__(self):
        self.ops = []
        self.lastw = {}
        self.readers = {}
        self.stream_last = {}
        self.last_on_eng = {}

    def begin_record(self):
        self.rec = []

    def end_record(self):
        r, self.rec = self.rec, None
        return r

    def replay_interleaved(self, lists):
        n = max(len(x) for x in lists)
        for j in range(n):
            for x in lists:
                if j < len(x):
                    a, kw = x[j]
                    self.add(*a, **kw)

    def add(self, eng, fn, reads=(), writes=(), dma=None, inc=16):
        if getattr(self, "rec", None) is not None:
            self.rec.append(((eng, fn), dict(reads=list(reads), writes=list(writes), dma=dma, inc=inc)))
            return None
        op = Op(eng, fn, dma, inc)
        deps = set()
        for k in reads:
            w = self.lastw.get(k)
            if w is not None:
                deps.add(w)
        for k in writes:
            w = self.lastw.get(k)
            if w is not None:
                deps.add(w)
            for r in self.readers.get(k, ()):
                deps.add(r)
        if dma is not None:
            p = self.stream_last.get(dma)
            if p is not None:
                deps.add(p)
            self.stream_last[dma] = op
        for k in writes:
            self.lastw[k] = op
            self.readers[k] = []
        for k in reads:
            if k not in writes:
                self.readers.setdefault(k, []).append(op)
        deps.discard(op)
        op.deps = list(deps)
        self.ops.append(op)
        self.last_on_eng[eng] = op
        return op

    def barrier(self):
        pend = list(self.last_on_eng.values()) + [v for k, v in self.stream_last.items()
                                                  if not (k.startswith("cc") and k.endswith("_1"))]
        for e in ENGS:
            op = Op(e, None, None, 0)
            op.deps = [d for d in pend]
            self.ops.append(op)

    def emit(self, nc, block, es):
        for op in self.ops:
            for d in op.deps:
                if d.eng == "pe" and op.eng == "pe" and d.dma is None and op.dma is None:
                    continue
                d.signal = True
        sems = {e: es.enter_context(nc.semaphore("s_" + e)) for e in ENGS}
        ssem = {}
        cnt = {e: 0 for e in ENGS}
        scnt = {}
        for op in self.ops:
            if op.fn is None:
                continue
            if op.dma is not None:
                if op.dma not in ssem:
                    ssem[op.dma] = es.enter_context(nc.semaphore("d_" + op.dma))
                    scnt[op.dma] = 0
                scnt[op.dma] += op.inc
                op.tok = (ssem[op.dma], scnt[op.dma])
                op.signal = True
            elif op.signal:
                cnt[op.eng] += 1
                op.tok = (sems[op.eng], cnt[op.eng])
        per = {e: [o for o in self.ops if o.eng == e] for e in ENGS}

        def run(eng_name, eng):
            waited = {}
            for op in per[eng_name]:
                for d in op.deps:
                    if d.tok is None:
                        continue
                    if d.eng == "pe" and eng_name == "pe" and d.dma is None:
                        continue
                    s, v = d.tok
                    if waited.get(id(s), 0) >= v:
                        continue
                    eng.wait_ge(s, v)
                    waited[id(s)] = v
                if op.fn is None:
                    continue
                ins = op.fn(eng)
                if op.signal:
                    if op.dma is not None and op.dma.startswith("cc"):
                        ins.then_inc(op.tok[0])
                    else:
                        ins.then_inc(op.tok[0], op.inc if op.dma is not None else 1)

        block.tensor(lambda e: run("pe", e))
        block.scalar(lambda e: run("act", e))
        block.vector(lambda e: run("dve", e))
        block.gpsimd(lambda e: run("pool", e))
        block.sync(lambda e: run("sp", e))


def build(n_layers=DEPTH, stop_after=None, dbg=None, ne_decl=NE):
    nc = bass.Bass("TRN2", target_bir_lowering=False)
    P = Prog()
    es = ExitStack()

    def din(name, shape, dt=F32):
        return nc.dram_tensor(name, list(shape), dt, kind="ExternalInput")

    x_in = din("x", [NB, 128, D])
    w_in = din("w_in", [DEPTH, D, 3584])
    bfm = din("bfm", [DEPTH, 128, 24])
    bv = din("bv", [DEPTH, 128, 512])
    cosT = din("cosT", [128, T])
    sinT = din("sinT", [128, T])
    convp = din("convp", [DEPTH, 128, 4, 34])
    lamv = din("lamv", [DEPTH, 128, 4, 64])
    gsub = din("gsub", [DEPTH, 128, 128])
    w_out = din("w_out", [DEPTH, D, D])
    lnp = din("lnp", [DEPTH, 4, 128, D])
    w_r = din("w_r", [DEPTH, D, 36])
    b_r = din("b_r", [DEPTH, 128, 36])
    w_g = din("w_gate_e", [DEPTH, ne_decl, D, 512])
    w_u = din("w_up_e", [DEPTH, ne_decl, D, 512])
    w_d = din("w_down_e", [DEPTH, ne_decl, 512, D])
    ident_in = din("ident", [128, 128])
    tri_in = din("tri", [128, 128])
    ec_in = din("ec", [128, NE])
    maskb_in = din("maskb", [128, 4, 128])
    hcoef_in = din("hcoef", [128, 4])
    out = nc.dram_tensor("out", [NB, 128, D], F32, kind="ExternalOutput")
    dbg_out = {}
    if dbg is not None:
        for (dn, dfn, dshape, ddt) in dbg:
            dbg_out[dn] = nc.dram_tensor("dbg_" + dn, list(dshape), ddt, kind="ExternalOutput")

    KTb = [nc.dram_tensor("KTb%d" % a, [256, T], BF16) for a in range(2)]
    KTg = [nc.dram_tensor("KTg%d" % a, [1024, T], BF16) for a in range(2)]
    Vb = [nc.dram_tensor("Vb%d" % a, [T, 256], BF16) for a in range(2)]
    Vg = [nc.dram_tensor("Vg%d" % a, [4 * T, 256], BF16) for a in range(2)]
    Hb = nc.dram_tensor("Hb", [512, 512], F32)
    Hg = nc.dram_tensor("Hg", [2048, 512], F32)
    xs = nc.dram_tensor("xs", [NE * CAP + 1, D], BF16)
    ys = nc.dram_tensor("ys", [NE * CAP * 2 + 2, 512], F32)

    def sb(name, shape, dt):
        return es.enter_context(nc.sbuf_tensor("sb_" + name, list(shape), dt))

    X = sb("X", [128, NB, D], F32)
    R1 = sb("R1", [128, 16384], BF16)
    R2 = sb("R2", [128, 8192], F32)
    R3 = sb("R3", [128, 8192], BF16)
    R4 = sb("R4", [128, 8192], F32)
    PT = sb("PT", [128, 4, 512], BF16)
    SC = sb("SC", [128, 3072], F32)
    ident = sb("ident", [128, 128], F32)
    identb = sb("identb", [128, 128], BF16)
    trib = sb("trib", [128, 128], BF16)
    onesb = sb("onesb", [128, 128], BF16)
    onesf = sb("onesf", [128, 128], F32)
    ecs = sb("ecs", [128, NE], F32)
    maskb = sb("maskb", [128, 4, 128], BF16)
    hcoef = sb("hcoef", [128, 4], F32)
    bfm_s = sb("bfm_s", [128, 24], F32)
    convp_s = sb("convp_s", [128, 4, 34], F32)
    gsub_s = sb("gsub_s", [128, 128], F32)
    br_s = sb("br_s", [128, 36], F32)
    wr_s = sb("wr_s", [128, 8, 36], F32)
    small = sb("small", [128, 64], F32)
    epsT = sb("epsT", [128, 1], F32)
    lnsm = sb("lnsm", [128, 2, 20], F32)
    destI = sb("destI", [128, NB, 2], I32)
    destG = sb("destG", [128, NB, 4], I32)
    wgt = sb("wgt", [128, NB, 2], F32)
    selB = sb("selB", [128, NB, NE], BF16)

    Y1 = sb("Y1", [128, 512], F32)
    zt = sb("zt", [128, D], BF16)
    ps = [es.enter_context(nc.psum_tensor("ps%d" % i, [128, 512], F32)) for i in range(8)]

    xT = R1[:, :].rearrange("p (k t) -> p k t", k=8)
    mergedT = xT
    uT = R2[:, :].rearrange("p (c t) -> p c t", c=4)
    KTh = [R2[:, :].bitcast(BF16)[:, s * 8192:(s + 1) * 8192].rearrange("p (r t) -> p r t", r=4)
           for s in range(2)]
    QT = R3[:, :].rearrange("p (h t) -> p h t", h=4)
    R4b = R4[:, :].bitcast(BF16)
    Vh = R4b[:, 0:64 * 129].rearrange("p (j e) -> p j e", e=129)
    Win = [R4b[:, s * 2048:(s + 1) * 2048].rearrange("p (k n) -> p k n", k=8) for s in range(3)]
    cosA = R4[:, 3072:5120]
    sinA = R4[:, 5120:7168]
    HG = R4[:, 3072:5120].rearrange("p (r c) -> p r c", r=4)
    ext = R4[:, 5120:7680].rearrange("p (i c) -> p i c", i=16)
    tmpA = R4[:, 7680:8192]
    lam_s = R4[:, 7936:8192].rearrange("p (a d) -> p a d", a=4)
    Qph = SC[:, 0:2048].bitcast(BF16).rearrange("p (m t) -> p m t", m=2)
    scr = [SC[:, 2048 + i * 512: 2048 + (i + 1) * 512] for i in range(2)]
    scq = [SC[:, i * 512:(i + 1) * 512] for i in range(4)]

    bank_ctr = [0]

    def bank():
        b = bank_ctr[0] % 8
        bank_ctr[0] += 1
        return b

    P.add("sp", lambda e: e.dma_start(out=ident[:], in_=ident_in[:, :]), writes=["ident"], dma="c0")
    P.add("pool", lambda e: e.dma_start(out=identb[:], in_=ident_in[:, :]), writes=["identb"], dma="c1")
    P.add("pool", lambda e: e.dma_start(out=trib[:], in_=tri_in[:, :]), writes=["trib"], dma="c1")
    P.add("pool", lambda e: e.dma_start(out=maskb[:], in_=maskb_in[:, :, :]), writes=["maskb"], dma="c1")
    P.add("sp", lambda e: e.dma_start(out=ecs[:], in_=ec_in[:, :]), writes=["ecs"], dma="c0")
    P.add("sp", lambda e: e.dma_start(out=hcoef[:], in_=hcoef_in[:, :]), writes=["hcoef"], dma="c0")
    P.add("pool", lambda e: e.memset(onesb[:], 1.0), writes=["onesb"])
    P.add("pool", lambda e: e.memset(onesf[:], 1.0 / 512.0), writes=["onesf"])
    P.add("pool", lambda e: e.memset(epsT[:], LN_EPS), writes=["epsT"])
    P.add("pool", lambda e: e.memset(Y1[:, :], 0.0), writes=["y1"])
    P.add("sp", lambda e: e.dma_start(out=ys[NE * CAP * 2:NE * CAP * 2 + 2, :], in_=Y1[0:2, 0:512]), reads=["y1"], writes=["ys"], dma="c0")
    for i in range(NB):
        P.add("sp", lambda e, i=i: e.dma_start(out=X[:, i, :], in_=x_in[i, :, :]), writes=["X%d" % i], dma="xl%d" % (i % 4))
    P.barrier()

    def ln_block(l, i, which, src_key_extra=()):
        g_t = lnG[which]
        b_t = lnB[which]
        xi = X[:, i, :]
        q = i % 2
        sm_ = lnsm[:, q, :]
        kq = "ln%d" % q
        st = sm_[:, 0:12].rearrange("p (a b) -> p a b", a=2)
        P.add("dve", lambda e: e.bn_stats(out=st[:, 0, :], in_=X[:, i, 0:512]), reads=["X%d" % i], writes=[kq])
        P.add("dve", lambda e: e.bn_stats(out=st[:, 1, :], in_=X[:, i, 512:1024]), reads=["X%d" % i], writes=[kq])
        P.add("dve", lambda e: e.bn_aggr(out=sm_[:, 12:14], in_=st), reads=[kq], writes=[kq])
        P.add("act", lambda e: e.activation(out=sm_[:, 14:15], in_=sm_[:, 13:14], func=AF.Ln, bias=epsT[:, 0:1]),
              reads=[kq, "epsT"], writes=[kq])
        P.add("act", lambda e: e.activation(out=sm_[:, 15:16], in_=sm_[:, 14:15], func=AF.Exp, scale=-0.5),
              reads=[kq], writes=[kq])
        P.add("dve", lambda e: e.scalar_tensor_tensor(out=sm_[:, 16:17], in0=sm_[:, 12:13], scalar=-1.0, in1=sm_[:, 15:16],
                                                      op0=ALU.mult, op1=ALU.mult),
              reads=[kq], writes=[kq])
        P.add("act", lambda e: e.activation(out=xi, in_=xi, func=AF.Identity, scale=sm_[:, 15:16], bias=sm_[:, 16:17]),
              reads=["X%d" % i, kq], writes=["X%d" % i])
        P.add("dve", lambda e: e.tensor_tensor(out=xi, in0=xi, in1=g_t, op=ALU.mult),
              reads=["X%d" % i, "lnG"], writes=["X%d" % i])
        P.add("pool" if which == 0 else "dve", lambda e: e.tensor_tensor(out=xi, in0=xi, in1=b_t, op=ALU.add),
              reads=["X%d" % i, "lnB"], writes=["X%d" % i])

    lnG = {}
    lnB = {}

    for l in range(n_layers):
        lam_init = 0.8 - 0.6 * math.exp(-0.3 * l)
        P.add("sp", lambda e, l=l: e.dma_start(out=bfm_s[:], in_=bfm[l, :, :]), writes=["bfm"], dma="c0")
        P.add("sp", lambda e, l=l: e.dma_start(out=convp_s[:], in_=convp[l, :, :, :]), writes=["convp"], dma="c0")
        P.add("sp", lambda e, l=l: e.dma_start(out=lam_s[:], in_=lamv[l, :, :, :]), writes=["lam"], dma="c0")
        P.add("sp", lambda e, l=l: e.dma_start(out=gsub_s[:], in_=gsub[l, :, :]), writes=["gsub"], dma="c0")
        P.add("sp", lambda e, l=l: e.dma_start(out=br_s[:], in_=b_r[l, :, :]), writes=["br"], dma="c0")
        P.add("sp", lambda e, l=l: e.dma_start(out=wr_s[:], in_=w_r[l].rearrange("(k p) n -> p k n", p=128)),
              writes=["wr"], dma="c0")
        P.add("dve", lambda e: e.tensor_tensor(out=lam_s[:, 0, :], in0=lam_s[:, 0, :], in1=lam_s[:, 1, :], op=ALU.mult),
              reads=["lam"], writes=["lam"])
        P.add("dve", lambda e: e.tensor_tensor(out=lam_s[:, 2, :], in0=lam_s[:, 2, :], in1=lam_s[:, 3, :], op=ALU.mult),
              reads=["lam"], writes=["lam"])
        P.add("dve", lambda e: e.tensor_reduce(out=small[:, 18:19], in_=lam_s[:, 0, :], axis=AX.X, op=ALU.add),
              reads=["lam"], writes=["l1"])
        P.add("dve", lambda e: e.tensor_reduce(out=small[:, 19:20], in_=lam_s[:, 2, :], axis=AX.X, op=ALU.add),
              reads=["lam"], writes=["l2"])
        P.add("act", lambda e: e.activation(out=small[:, 18:20], in_=small[:, 18:20], func=AF.Exp),
              reads=["l1", "l2"], writes=["l1", "l2"])
        P.add("dve", lambda e: e.tensor_tensor(out=small[:, 16:17], in0=small[:, 18:19], in1=small[:, 19:20], op=ALU.subtract),
              reads=["l1", "l2"], writes=["lamv"])
        P.add("dve", lambda e, li=lam_init: e.tensor_scalar(out=small[:, 17:18], in0=small[:, 16:17], scalar1=li, scalar2=-1.0,
                                                          op0=ALU.add, op1=ALU.mult),
              reads=["lamv"], writes=["nlam"])
        P.add("dve", lambda e, li=lam_init: e.tensor_scalar(out=gsub_s[:], in0=gsub_s[:], scalar1=(1.0 - li), scalar2=None,
                                                          op0=ALU.mult),
              reads=["gsub"], writes=["gsub"])

        P.add("sp", lambda e: e.dma_start(out=cosA, in_=cosT[:, :]), writes=["cosS"], dma="cs")
        P.add("sp", lambda e: e.dma_start(out=sinA, in_=sinT[:, :]), writes=["sinS"], dma="sn")
        for i in range(NB):
            for kk in range(2):
                b = bank()
                for j in range(4):
                    k = kk * 4 + j
                    P.add("pe", lambda e, b=b, j=j, i=i, k=k: e.transpose(out=ps[b][:, j * 128:(j + 1) * 128],
                                                                         in_=X[:, i, k * 128:(k + 1) * 128], identity=ident[:]),
                          reads=["X%d" % i, "ident"], writes=["ps%d" % b])
                eng = "dve" if (i + kk) % 2 == 0 else "act"
                if eng == "dve":
                    P.add("dve", lambda e, b=b, i=i, kk=kk: e.tensor_copy(
                        out=xT[:, kk * 4:(kk + 1) * 4, i * 128:(i + 1) * 128],
                        in_=ps[b][:, :].rearrange("p (a t) -> p a t", a=4)),
                        reads=["ps%d" % b], writes=["xT%d" % i])
                else:
                    P.add("act", lambda e, b=b, i=i, kk=kk: e.copy(
                        out=xT[:, kk * 4:(kk + 1) * 4, i * 128:(i + 1) * 128],
                        in_=ps[b][:, :].rearrange("p (a t) -> p a t", a=4)),
                        reads=["ps%d" % b], writes=["xT%d" % i])
        xT_keys = ["xT%d" % i for i in range(NB)]

        def load_w(g, l=l):
            s = g % 3
            P.add("pool", lambda e: e.dma_start(out=Win[s], in_=w_in[l].rearrange("(k p) n -> p k n", p=128)[:, :, g * 256:(g + 1) * 256]),
                  writes=["Win%d" % s], dma="win%d" % s)

        load_w(0)
        load_w(1)
        for g in range(14):
            if g + 2 < 14:
                load_w(g + 2)
            s = g % 3
            W = Win[s]
            wk = "Win%d" % s
            if g < 12:
                for tt in range(4):
                    tsl = slice(tt * 512, (tt + 1) * 512)
                    bA, bB = bank(), bank()
                    for half, b in ((0, bA), (1, bB)):
                        for k in range(8):
                            P.add("pe", lambda e, b=b, k=k, half=half, W=W, tsl=tsl: e.matmul(
                                ps[b][:, :], lhsT=W[:, k, half * 128:(half + 1) * 128], rhs=xT[:, k, tsl],
                                start=(k == 0), stop=(k == 7)),
                                reads=[wk] + xT_keys[tt * 4:(tt + 1) * 4], writes=["ps%d" % b])
                    if g < 4:
                        c = g
                        P.add("act", lambda e, bB=bB, c=c: e.activation(out=scq[0], in_=ps[bB][:, :], func=AF.Sigmoid,
                                                                         bias=bfm_s[:, 2 * c + 1:2 * c + 2]),
                              reads=["ps%d" % bB, "bfm"], writes=["scq0"])
                        P.add("dve", lambda e, bA=bA, c=c, tsl=tsl: e.scalar_tensor_tensor(
                            out=uT[:, c, tsl], in0=ps[bA][:, :], scalar=bfm_s[:, 2 * c:2 * c + 1], in1=scq[0],
                            op0=ALU.add, op1=ALU.mult),
                            reads=["ps%d" % bA, "scq0", "bfm"], writes=["uT%d" % c])
                    else:
                        hh = (g - 4) // 2
                        isk = (g - 4) % 2
                        ch = 8 + 2 * (g - 4)
                        cosS = cosA[:, tsl]
                        sinS = sinA[:, tsl]
                        P.add("dve", lambda e, bA=bA, ch=ch, cosS=cosS: e.scalar_tensor_tensor(
                            out=scq[1], in0=ps[bA][:, :], scalar=bfm_s[:, ch:ch + 1], in1=cosS, op0=ALU.add, op1=ALU.mult) if True else None,
                            reads=["ps%d" % bA, "cosS", "bfm"], writes=["scq1"])
                        P.add("dve", lambda e, bB=bB, ch=ch, sinS=sinS: e.scalar_tensor_tensor(
                            out=scq[2], in0=ps[bB][:, :], scalar=bfm_s[:, ch + 1:ch + 2], in1=sinS, op0=ALU.add, op1=ALU.mult),
                            reads=["ps%d" % bB, "sinS", "bfm"], writes=["scq2"])
                        if isk == 0:
                            P.add("pool", lambda e, hh=hh, tsl=tsl: e.tensor_tensor(out=QT[:, hh, tsl], in0=scq[1], in1=scq[2], op=ALU.add),
                                  reads=["scq1", "scq2"], writes=["QT%d" % hh])
                        else:
                            kst = scq[3].bitcast(BF16)[:, 0:512]
                            P.add("pool", lambda e, kst=kst: e.tensor_tensor(out=kst, in0=scq[1], in1=scq[2], op=ALU.add),
                                  reads=["scq1", "scq2"], writes=["kst"])
                            P.add("sp", lambda e, hh=hh, tsl=tsl, kst=kst: e.dma_start(out=KTb[hh // 2][(hh % 2) * 128:(hh % 2 + 1) * 128, tsl], in_=kst),
                                  reads=["kst"], writes=["KTb%d" % (hh // 2)], dma="kst")
            else:
                vh = g - 12
                for i in range(NB):
                    b = bank()
                    for k in range(8):
                        P.add("pe", lambda e, b=b, k=k, i=i, W=W: e.matmul(
                            ps[b][:, 0:256], lhsT=xT[:, k, i * 128:(i + 1) * 128], rhs=W[:, k, :],
                            start=(k == 0), stop=(k == 7)),
                            reads=[wk, "xT%d" % i], writes=["ps%d" % b])
                    vst = scq[i % 2].bitcast(BF16)[:, 0:256]
                    P.add("dve", lambda e, b=b, vst=vst, vh=vh: e.tensor_tensor(out=vst, in0=ps[b][:, 0:256],
                                                                               in1=tmpA[:, vh * 256:(vh + 1) * 256], op=ALU.add),
                          reads=["ps%d" % b, "bvS"], writes=["scq%d" % (i % 2)])
                    P.add("sp", lambda e, i=i, vh=vh, vst=vst: e.dma_start(out=Vb[vh][i * 128:(i + 1) * 128, :], in_=vst),
                          reads=["scq%d" % (i % 2)], writes=["Vb%d" % vh], dma="vst%d" % (i % 2))
            if g == 3:
                for c in range(4):
                    P.add("sp", lambda e, c=c: e.dma_start(
                        out=Hb[c * 128:(c + 1) * 128, :].rearrange("p (i j) -> p i j", j=32),
                        in_=uT[:, c, :].rearrange("p (i t) -> p i t", t=128)[:, :, 96:128]),
                        reads=["uT%d" % c], writes=["Hb"], dma="hb")
                P.add("pool", lambda e: e.collective_compute("AllGather", ALU.bypass, replica_groups=RG,
                                                             ins=[Hb.ap().opt()], outs=[Hg.ap().opt()]),
                      reads=["Hb"], writes=["Hg"], dma="ccH%d" % l, inc=1)
            if g == 10:
                P.add("sp", lambda e, l=l: e.dma_start(out=tmpA, in_=bv[l, :, :]), writes=["bvS", "lam"], dma="c0")
        P.barrier()
        if stop_after == "B":
            break

        for a in range(2):
            P.add("pool", lambda e, a=a: e.collective_compute("AllGather", ALU.bypass, replica_groups=RG,
                                                              ins=[KTb[a].ap().opt()], outs=[KTg[a].ap().opt()]),
                  reads=["KTb%d" % a], writes=["KTg%d" % a], dma="ccK%d_%d" % (l, a), inc=1)
            P.add("pool", lambda e, a=a: e.collective_compute("AllGather", ALU.bypass, replica_groups=RG,
                                                              ins=[Vb[a].ap().opt()], outs=[Vg[a].ap().opt()]),
                  reads=["Vb%d" % a], writes=["Vg%d" % a], dma="ccV%d_%d" % (l, a), inc=1)

        HG4 = HG.rearrange("p r (i j) -> p r i j", j=32)
        SCQ = ["scq0", "scq1", "scq2", "scq3"]
        extb = [R4[:, 5120:6400].bitcast(BF16).rearrange("p (i c) -> p i c", i=16),
                R4[:, 6400:7680].bitcast(BF16).rearrange("p (i c) -> p i c", i=16)]
        dgs = [R4[:, 0:1984].bitcast(BF16).rearrange("p (k n) -> p k n", k=31),
               SC[:, 0:1984].bitcast(BF16).rearrange("p (k n) -> p k n", k=31)]
        def conv_prep(c):
            q2 = c % 2
            ex_ = extb[q2]
            dg = dgs[q2]
            ek = "extb%d" % q2
            dk = ["dg%d" % q2] + (SCQ if q2 == 1 else ["Win0", "Win1"])
            halo = ex_[:, :, 0:32]
            P.add("sp", lambda e, c=c: e.dma_start(out=HG, in_=Hg.ap().rearrange("(r q) n -> q r n", r=4)[c * 128:(c + 1) * 128, :, :]),
                  reads=["Hg"], writes=["HG"], dma="hg")
            for k in range(31):
                P.add("act", lambda e, c=c, k=k, dg=dg: e.activation(out=dg[:, k, :], in_=identb[:], func=AF.Identity,
                                                                     scale=convp_s[:, c, k:k + 1]),
                      reads=["identb", "convp"], writes=dk)
            P.add("dve", lambda e, ex_=ex_: e.memset(ex_[:, 0, 0:32], 0.0), writes=[ek])
            P.add("dve", lambda e, ex_=ex_: e.tensor_scalar(out=ex_[:, 1:16, 0:32], in0=HG4[:, 3, 0:15, :], scalar1=hcoef[:, 0:1],
                                                            scalar2=None, op0=ALU.mult),
                  reads=["HG", "hcoef"], writes=[ek])
            for q in range(1, 4):
                P.add("dve", lambda e, q=q, halo=halo: e.scalar_tensor_tensor(out=halo, in0=HG4[:, q - 1, :, :], scalar=hcoef[:, q:q + 1],
                                                                              in1=halo, op0=ALU.mult, op1=ALU.add),
                      reads=["HG", "hcoef"], writes=[ek])
            accv = uT[:, c, :].rearrange("p (i t) -> p i t", t=128)
            P.add("act", lambda e, accv=accv, ex_=ex_: e.copy(out=ex_[:, :, 32:160], in_=accv), reads=["uT%d" % c], writes=[ek])

        def conv_compute(c):
            q2 = c % 2
            ex_ = extb[q2]
            dg = dgs[q2]
            ek = "extb%d" % q2
            for tt in range(4):
                b = bank()
                for k in range(31):
                    P.add("pe", lambda e, b=b, k=k, tt=tt, dg=dg, ex_=ex_: e.matmul(
                        ps[b][:, :].rearrange("p (i t) -> p i t", i=4), lhsT=dg[:, k, :],
                        rhs=ex_[:, tt * 4:(tt + 1) * 4, 2 + k:130 + k], start=(k == 0), stop=(k == 30)),
                        reads=[ek, "dg%d" % q2], writes=["ps%d" % b])
                if tt % 2 == 0:
                    P.add("act", lambda e, b=b, c=c, tt=tt: e.activation(out=uT[:, c, tt * 512:(tt + 1) * 512], in_=ps[b][:, :],
                                                                         func=AF.Identity, bias=convp_s[:, c, 31:32]),
                          reads=["ps%d" % b, "convp"], writes=["uT%d" % c])
                else:
                    P.add("dve", lambda e, b=b, c=c, tt=tt: e.tensor_scalar(out=uT[:, c, tt * 512:(tt + 1) * 512], in0=ps[b][:, :],
                                                                            scalar1=convp_s[:, c, 31:32], scalar2=None, op0=ALU.add),
                          reads=["ps%d" % b, "convp"], writes=["uT%d" % c])

        conv_prep(0)
        for c in range(4):
            if c + 1 < 4:
                conv_prep(c + 1)
            conv_compute(c)
        UT = ["uT%d" % c for c in range(4)]
        for tt in range(4):
            tsl = slice(tt * 512, (tt + 1) * 512)
            bm = bank()
            for c in range(4):
                P.add("pe", lambda e, bm=bm, c=c, tsl=tsl: e.matmul(ps[bm][:, :], lhsT=onesf[:], rhs=uT[:, c, tsl],
                                                                     start=(c == 0), stop=(c == 3)),
                      reads=["uT%d" % c, "onesf"], writes=["ps%d" % bm])
            for c in range(4):
                P.add("dve", lambda e, bm=bm, c=c, tsl=tsl: e.tensor_tensor(out=uT[:, c, tsl], in0=uT[:, c, tsl], in1=ps[bm][:, :],
                                                                            op=ALU.subtract),
                      reads=["ps%d" % bm], writes=["uT%d" % c])
            bq = bank()
            for c in range(4):
                P.add("act", lambda e, c=c, tsl=tsl: e.activation(out=scq[c], in_=uT[:, c, tsl], func=AF.Square),
                      reads=["uT%d" % c], writes=["scq%d" % c])
                P.add("pe", lambda e, bq=bq, c=c: e.matmul(ps[bq][:, :], lhsT=onesf[:], rhs=scq[c], start=(c == 0), stop=(c == 3)),
                      reads=["scq%d" % c, "onesf"], writes=["ps%d" % bq])
            P.add("act", lambda e, bq=bq: e.activation(out=scr[0], in_=ps[bq][:, :], func=AF.Ln, bias=epsT[:, 0:1]),
                  reads=["ps%d" % bq, "epsT"], writes=["scr0"])
            P.add("act", lambda e: e.activation(out=scr[0], in_=scr[0], func=AF.Exp, scale=-0.5), reads=["scr0"], writes=["scr0"])
            for c in range(4):
                P.add("dve", lambda e, c=c, tsl=tsl: e.tensor_tensor(out=scq[c], in0=uT[:, c, tsl], in1=scr[0], op=ALU.mult),
                      reads=["uT%d" % c, "scr0"], writes=["scq%d" % c])
                P.add("act", lambda e, c=c, tsl=tsl: e.activation(out=mergedT[:, c, tsl], in_=scq[c], func=AF.Silu,
                                                                  scale=convp_s[:, c, 32:33], bias=convp_s[:, c, 33:34]),
                      reads=["scq%d" % c, "convp"], writes=["mT%d" % c])
        P.barrier()
        if stop_after == "D":
            break

        P.add("pool", lambda e: e.memset(Vh[:, :, 128:129], 1.0), writes=["Vh"])
        P.add("pool", lambda e: e.memset(Qph[64:128, 0, :], 0.0), writes=["Qph"])
        P.add("pool", lambda e: e.memset(Qph[0:64, 1, :], 0.0), writes=["Qph"])
        for h in range(4):
            s = h % 2
            P.add("sp", lambda e, h=h, s=s: e.dma_start(out=KTh[s], in_=KTg[h // 2].ap().rearrange("(r q) t -> q r t", r=4)[(h % 2) * 128:(h % 2 + 1) * 128, :, :]),
                  reads=["KTg%d" % (h // 2)], writes=["KTh%d" % s], dma="kth%d" % s)
            P.add("sp", lambda e, h=h: e.dma_start(out=Vh[:, :, 0:128], in_=Vg[h // 2].ap().rearrange("(j p) e -> p j e", p=128)[:, :, (h % 2) * 128:(h % 2 + 1) * 128]),
                  reads=["Vg%d" % (h // 2)], writes=["Vh"], dma="vh")
            P.add("pool", lambda e, h=h: e.tensor_copy(out=Qph[0:64, 0, :], in_=QT[0:64, h, :]), reads=["QT%d" % h], writes=["Qph"])
            P.add("act", lambda e, h=h: e.copy(out=Qph[64:128, 1, :], in_=QT[64:128, h, :]), reads=["QT%d" % h], writes=["Qph"])
            if l == 0 and h == 0:
                P.add("pool", lambda e: e.memset(zt[:, :], 0.0), writes=["zt"])
                P.add("pool", lambda e: e.dma_start(
                    out=xs[0:NE * CAP, :].rearrange("(n p) d -> p n d", p=128),
                    in_=zt[:, :].rearrange("p (o d) -> p o d", o=1).to_broadcast([128, NE * CAP // 128, D])),
                    reads=["zt"], writes=["xs"], dma="z0")
                P.add("pool", lambda e: e.dma_start(out=xs[NE * CAP:NE * CAP + 1, :], in_=zt[0:1, :]), reads=["zt"], writes=["xs"], dma="z0")
            tiles = [(i, pp) for i in range(NB) for pp in range(2 * i + 2)]
            LA = 3

            def qk(n, h=h, s=s):
                i, pp = tiles[n]
                b = n % 3
                for kbp in range(2):
                    jb = 2 * pp + kbp
                    rr, il = jb % 4, jb // 4
                    masked = jb >= 4 * i
                    P.add("pe", lambda e, b=b, kbp=kbp, rr=rr, il=il, i=i, masked=masked: e.matmul(
                        ps[b][:, kbp * 256:(kbp + 1) * 256].rearrange("p (m q) -> p m q", m=2),
                        lhsT=KTh[s][:, rr, il * 128:(il + 1) * 128], rhs=Qph[:, :, i * 128:(i + 1) * 128],
                        start=True, stop=(not masked)),
                        reads=["KTh%d" % s, "Qph"], writes=["ps%d" % b])
                    if masked:
                        for m in range(2):
                            osl = slice((kbp * 2 + m) * 128, (kbp * 2 + m + 1) * 128)
                            P.add("pe", lambda e, b=b, osl=osl, q=jb - 4 * i, m=m: e.matmul(
                                ps[b][:, osl], lhsT=identb[:], rhs=maskb[:, q, :], start=False, stop=(m == 1)),
                                reads=["identb", "maskb"], writes=["ps%d" % b])
                P.add("act", lambda e, b=b, n=n: e.activation(out=PT[:, n % 4, :], in_=ps[b][:, :], func=AF.Exp, scale=0.125),
                      reads=["ps%d" % b], writes=["PT%d" % (n % 4)])

            def pv(n, h=h):
                i, pp = tiles[n]
                ob = 4 + i % 2
                for kbp in range(2):
                    jb = 2 * pp + kbp
                    rr, il = jb % 4, jb // 4
                    for m in range(2):
                        osl = slice((kbp * 2 + m) * 128, (kbp * 2 + m + 1) * 128)
                        P.add("pe", lambda e, ob=ob, m=m, n=n, osl=osl, rr=rr, il=il, first=(pp == 0 and kbp == 0),
                              last=(pp == 2 * i + 1 and kbp == 1): e.matmul(
                            ps[ob + 2 * m][:, 0:129], lhsT=PT[:, n % 4, osl], rhs=Vh[:, rr * 16 + il, :],
                            start=first, stop=last),
                            reads=["PT%d" % (n % 4), "Vh"], writes=["ps%d" % (ob + 2 * m)])
                if pp == 2 * i + 1:
                    pending.append(epilogue_stages(i, h))

            def epilogue_stages(i, h=h):
                ob = 4 + i % 2
                O = ps[ob]
                OB = ps[ob + 2]
                okey = "ps%d" % ob
                okeyB = "ps%d" % (ob + 2)
                tb = 3
                sA = scr[i % 2]
                sk = "scr%d" % (i % 2)
                sm = small[:, 32 + 8 * (i % 2):40 + 8 * (i % 2)]
                mk = "sm%d" % (i % 2)
                onb = sA[:, 384:448].bitcast(BF16)

                def st1():
                    P.add("dve", lambda e: e.reciprocal(out=sm[:, 0:1], in_=O[:, 128:129]), reads=[okey], writes=[mk])
                    P.add("dve", lambda e: e.reciprocal(out=sm[:, 1:2], in_=OB[:, 128:129]), reads=[okeyB, mk], writes=[mk])
                    P.add("dve", lambda e: e.tensor_tensor(out=sm[:, 2:3], in0=sm[:, 1:2], in1=small[:, 17:18], op=ALU.mult),
                          reads=[mk, "nlam"], writes=[mk])
                    P.add("dve", lambda e: e.tensor_scalar(out=sA[:, 0:128], in0=O[:, 0:128], scalar1=sm[:, 0:1],
                                                           scalar2=None, op0=ALU.mult),
                          reads=[okey, mk], writes=[sk])
                    P.add("dve", lambda e: e.scalar_tensor_tensor(
                        out=sA[:, 128:256], in0=OB[:, 0:128], scalar=sm[:, 2:3], in1=sA[:, 0:128], op0=ALU.mult, op1=ALU.add),
                        reads=[okeyB, mk, sk], writes=[sk])
                    P.add("dve", lambda e: e.memset(sm[:, 3:4], 0.0), writes=[mk])
                    P.add("dve", lambda e: e.scalar_tensor_tensor(
                        out=sA[:, 256:384], in0=sA[:, 128:256], scalar=1.0, in1=sA[:, 128:256], op0=ALU.mult, op1=ALU.mult,
                        accum_out=sm[:, 3:4]),
                        reads=[sk, mk], writes=[sk, mk])

                def st2():
                    P.add("act", lambda e: e.activation(out=sm[:, 4:5], in_=sm[:, 3:4], func=AF.Ln, scale=1.0 / 128.0,
                                                        bias=epsT[:, 0:1]),
                          reads=[mk, "epsT"], writes=[mk])
                    P.add("act", lambda e: e.activation(out=sm[:, 5:6], in_=sm[:, 4:5], func=AF.Exp, scale=-0.5),
                          reads=[mk], writes=[mk])

                def st3():
                    P.add("dve", lambda e: e.scalar_tensor_tensor(
                        out=onb, in0=sA[:, 128:256], scalar=sm[:, 5:6], in1=gsub_s[:], op0=ALU.mult, op1=ALU.mult),
                        reads=[sk, mk, "gsub"], writes=[sk])
                    P.add("pe", lambda e: e.transpose(out=ps[tb][:, :].bitcast(BF16)[:, 0:128], in_=onb, identity=identb[:]),
                          reads=[sk, "identb"], writes=["ps%d" % tb])

                def st4():
                    P.add("dve", lambda e: e.tensor_copy(out=mergedT[:, 4 + h, i * 128:(i + 1) * 128],
                                                         in_=ps[tb][:, :].bitcast(BF16)[:, 0:128]),
                          reads=["ps%d" % tb], writes=["mT%d" % (4 + h)])
                return [st1, (lambda: None), (lambda: None), st2, (lambda: None), (lambda: None), st3, (lambda: None), st4]

            pending = []

            def advance():
                for stg in list(pending):
                    stg.pop(0)()
                    if not stg:
                        pending.remove(stg)

            for n in range(len(tiles) + LA):
                if n < len(tiles):
                    qk(n)
                advance()
                if n >= LA:
                    pv(n - LA)
            while pending:
                advance()
        P.barrier()
        if stop_after == "E":
            break

        woS = R2[:, :].bitcast(BF16)[:, 0:8192].rearrange("p (k n) -> p k n", k=8)
        lnT = R2[:, 4096:8192].rearrange("p (w d) -> p w d", w=4)
        lnG[0], lnB[0], lnG[1], lnB[1] = lnT[:, 0, :], lnT[:, 1, :], lnT[:, 2, :], lnT[:, 3, :]
        P.add("pool", lambda e, l=l: e.dma_start(out=woS, in_=w_out[l].rearrange("(k p) n -> p k n", p=128)), writes=["woS"], dma="wo")
        P.add("sp", lambda e, l=l: e.dma_start(out=lnT, in_=lnp[l].rearrange("w p d -> p w d")), writes=["lnG", "lnB"], dma="ln")
        x1b = [R4b[:, s * 1024:(s + 1) * 1024] for s in range(6)]
        x1T = R4[:, 1024:2048].rearrange("p (k t) -> p k t", k=8)
        rt = R4[:, 4096:4352]
        MT = ["mT%d" % k for k in range(8)]
        x1Ts = [R4[:, 3072:4096].rearrange("p (k t) -> p k t", k=8), R4[:, 4096:5120].rearrange("p (k t) -> p k t", k=8)]
        rts = [R4[:, 5120 + 256 * q:5376 + 256 * q] for q in range(6)]
        def pass1_block(i):
            bsl = slice(i * 128, (i + 1) * 128)
            xk = "X%d" % i
            for nt in range(2):
                b = bank()
                for k in range(8):
                    P.add("pe", lambda e, b=b, k=k, bsl=bsl, nt=nt: e.matmul(ps[b][:, :], lhsT=mergedT[:, k, bsl],
                                                                             rhs=woS[:, k, nt * 512:(nt + 1) * 512],
                                                                             start=(k == 0), stop=(k == 7)),
                          reads=["mT%d" % k, "woS"], writes=["ps%d" % b])
                P.add("dve", lambda e, b=b, i=i, nt=nt: e.scalar_tensor_tensor(
                    out=X[:, i, nt * 512:(nt + 1) * 512], in0=X[:, i, nt * 512:(nt + 1) * 512], scalar=ALPHA, in1=ps[b][:, :],
                    op0=ALU.mult, op1=ALU.add),
                    reads=["ps%d" % b], writes=[xk])
            ln_block(l, i, 0)

        p1 = []
        for i in range(NB):
            P.begin_record()
            pass1_block(i)
            p1.append(P.end_record())
        for i in range(0, NB, 2):
            P.replay_interleaved([p1[i], p1[i + 1]])

        P.barrier()
        R1w = R1[:, :]
        R2w = R2[:, :].bitcast(BF16)
        def _w8(ap):
            return ap.rearrange("p (k n) -> p k n", k=8)
        def _w4(ap):
            return ap.rearrange("p (k n) -> p k n", k=4)
        Wg = [_w8(R1w[:, 0:4096]), _w8(R1w[:, 12288:16384]), _w8(R2w[:, 0:4096])]
        Wu = [_w8(R1w[:, 4096:8192]), _w8(R3[:, 0:4096]), _w8(R2w[:, 4096:8192])]
        Wd = [_w4(R1w[:, 8192:12288]), _w4(R3[:, 4096:8192]), _w4(R2w[:, 8192:12288])]

        def load_expert_w(ex, l=l):
            s = ex % 3
            P.add("pool", lambda e: e.dma_start(out=Wg[s], in_=w_g[l, ex].rearrange("(k p) n -> p k n", p=128)),
                  writes=["Wg%d" % s], dma="wg%d" % s)
            P.add("pool", lambda e: e.dma_start(out=Wu[s], in_=w_u[l, ex].rearrange("(k p) n -> p k n", p=128)),
                  writes=["Wu%d" % s], dma="wu%d" % s)
            P.add("pool", lambda e: e.dma_start(out=Wd[s], in_=w_d[l, ex].rearrange("(k p) n -> p k n", p=128)),
                  writes=["Wd%d" % s], dma="wd%d" % s)

        if ne_decl == NE:
            for ex in range(3):
                load_expert_w(ex)

        def route_block(i, part):
            par = i % 2
            xk = "X%d" % i
            x1T = x1Ts[par]
            x1Tk = "x1T%d" % par
            rt = rts[i % 6]
            xb = x1b[i % 6]
            xbk = "x1b%d" % (i % 6)
            RT = ["rt%d" % (i % 6)]
            if part == 1:
                return route_chain(i, rt, RT, xb, xbk)
            P.add("act", lambda e: e.copy(out=xb, in_=X[:, i, :]), reads=[xk], writes=[xbk])
            for kk in range(2):
                b = bank()
                for j in range(4):
                    k = kk * 4 + j
                    P.add("pe", lambda e, b=b, j=j, k=k: e.transpose(out=ps[b][:, j * 128:(j + 1) * 128],
                                                                   in_=X[:, i, k * 128:(k + 1) * 128], identity=ident[:]),
                          reads=[xk, "ident"], writes=["ps%d" % b])
                if kk == 0:
                    P.add("dve", lambda e, b=b, kk=kk: e.tensor_copy(out=x1T[:, kk * 4:(kk + 1) * 4, :],
                                                                    in_=ps[b][:, :].rearrange("p (a t) -> p a t", a=4)),
                          reads=["ps%d" % b], writes=[x1Tk])
                else:
                    P.add("act", lambda e, b=b, kk=kk: e.copy(out=x1T[:, kk * 4:(kk + 1) * 4, :],
                                                             in_=ps[b][:, :].rearrange("p (a t) -> p a t", a=4)),
                          reads=["ps%d" % b], writes=[x1Tk])
            bl = bank()
            for k in range(8):
                P.add("pe", lambda e, bl=bl, k=k: e.matmul(ps[bl][:, 0:36], lhsT=x1T[:, k, :], rhs=wr_s[:, k, :],
                                                          start=(k == 0), stop=(k == 7)),
                      reads=[x1Tk, "wr"], writes=["ps%d" % bl])
            L = rt[:, 0:36]

            def dv(fn, extra_r=(), extra_w=()):
                P.add("dve", fn, reads=RT + list(extra_r), writes=RT + list(extra_w))

            P.add("dve", lambda e, bl=bl: e.tensor_tensor(out=rt[:, 0:36], in0=ps[bl][:, 0:36], in1=br_s[:], op=ALU.add),
                  reads=["ps%d" % bl, "br"], writes=RT)

        def route_chain(i, rt, RT, xb, xbk):
            def dv(fn, extra_r=(), extra_w=()):
                P.add("dve", fn, reads=RT + list(extra_r), writes=RT + list(extra_w))

            dv(lambda e: e.tensor_reduce(out=rt[:, 40:41], in_=rt[:, 0:4], axis=AX.X, op=ALU.max))
            dv(lambda e: e.tensor_tensor(out=rt[:, 44:48], in0=rt[:, 0:4], in1=rt[:, 40:41].to_broadcast([128, 4]), op=ALU.is_equal))
            dv(lambda e: e.tensor_scalar(out=rt[:, 41:42], in0=rt[:, 40:41], scalar1=-1.0, scalar2=None, op0=ALU.mult))
            dv(lambda e: e.memset(rt[:, 42:43], 0.0))
            P.add("act", lambda e: e.activation(out=rt[:, 48:52], in_=rt[:, 0:4], func=AF.Exp, bias=rt[:, 41:42], accum_out=rt[:, 42:43]),
                  reads=RT, writes=RT)
            dv(lambda e: e.reciprocal(out=rt[:, 43:44], in_=rt[:, 42:43]))
            dv(lambda e: e.tensor_scalar(out=rt[:, 52:56], in0=rt[:, 44:48], scalar1=-1.0, scalar2=1e9, op0=ALU.add, op1=ALU.mult))
            for g in range(4):
                dv(lambda e, g=g: e.tensor_scalar(out=rt[:, 64 + 8 * g:72 + 8 * g], in0=rt[:, 4 + 8 * g:12 + 8 * g],
                                                  scalar1=rt[:, 52 + g:53 + g], scalar2=None, op0=ALU.add))
            dv(lambda e: e.max(out=rt[:, 96:104], in_=rt[:, 64:96]))
            dv(lambda e: e.tensor_tensor(out=rt[:, 104:136], in0=rt[:, 64:96], in1=rt[:, 96:97].to_broadcast([128, 32]), op=ALU.is_equal))
            dv(lambda e: e.tensor_tensor(out=rt[:, 136:168], in0=rt[:, 64:96], in1=rt[:, 97:98].to_broadcast([128, 32]), op=ALU.is_ge))
            dv(lambda e, i=i: e.tensor_copy(out=selB[:, i, :], in_=rt[:, 136:168]), extra_w=["selB%d" % i])
            dv(lambda e: e.tensor_tensor(out=rt[:, 168:200], in0=rt[:, 136:168], in1=rt[:, 104:136], op=ALU.subtract))
            dv(lambda e: e.tensor_tensor(out=rt[:, 56:57], in0=rt[:, 97:98], in1=rt[:, 96:97], op=ALU.subtract))
            P.add("act", lambda e: e.activation(out=rt[:, 57:58], in_=rt[:, 56:57], func=AF.Exp), reads=RT, writes=RT)
            dv(lambda e: e.tensor_scalar(out=rt[:, 58:59], in0=rt[:, 57:58], scalar1=1.0, scalar2=None, op0=ALU.add))
            dv(lambda e: e.reciprocal(out=rt[:, 58:59], in_=rt[:, 58:59]))
            dv(lambda e: e.tensor_tensor(out=rt[:, 59:60], in0=rt[:, 58:59], in1=rt[:, 43:44], op=ALU.mult))
            dv(lambda e: e.tensor_tensor(out=rt[:, 60:61], in0=rt[:, 59:60], in1=rt[:, 57:58], op=ALU.mult))
            bc = bank()
            for ip in range(i + 1):
                P.add("pe", lambda e, bc=bc, ip=ip, i=i: e.matmul(ps[bc][:, 0:32], lhsT=(trib[:] if ip == i else onesb[:]),
                                                                  rhs=selB[:, ip, :], start=(ip == 0), stop=(ip == i)),
                      reads=["selB%d" % ip, "trib", "onesb"], writes=["ps%d" % bc])
            P.add("dve", lambda e, bc=bc: e.tensor_tensor(out=rt[:, 200:232], in0=ps[bc][:, 0:32], in1=ecs[:], op=ALU.add),
                  reads=["ps%d" % bc, "ecs"] + RT, writes=RT)
            P.add("dve", lambda e, bc=bc: e.tensor_scalar(out=rt[:, 4:36], in0=ps[bc][:, 0:32], scalar1=float(CAP), scalar2=None, op0=ALU.is_lt),
                  reads=["ps%d" % bc] + RT, writes=RT)
            dv(lambda e: e.tensor_scalar(out=rt[:, 200:232], in0=rt[:, 200:232], scalar1=-BIG, scalar2=None, op0=ALU.add))
            dv(lambda e: e.tensor_tensor(out=rt[:, 200:232], in0=rt[:, 200:232], in1=rt[:, 4:36], op=ALU.mult))
            dv(lambda e: e.tensor_scalar(out=rt[:, 200:232], in0=rt[:, 200:232], scalar1=BIG, scalar2=None, op0=ALU.add))
            for kx, so in ((0, 104), (1, 168)):
                dv(lambda e, so=so: e.tensor_tensor(out=rt[:, 64:96], in0=rt[:, so:so + 32], in1=rt[:, 200:232], op=ALU.mult))
                dv(lambda e, kx=kx: e.tensor_reduce(out=rt[:, 61 + kx:62 + kx], in_=rt[:, 64:96], axis=AX.X, op=ALU.add))
                dv(lambda e, kx=kx, i=i: e.tensor_copy(out=destI[:, i, kx:kx + 1], in_=rt[:, 61 + kx:62 + kx]), extra_w=["dest%d" % i])
                dv(lambda e, kx=kx: e.tensor_scalar(out=rt[:, 36 + kx:37 + kx], in0=rt[:, 61 + kx:62 + kx], scalar1=BIG - 0.5, scalar2=None,
                                                    op0=ALU.is_lt))
                dv(lambda e, kx=kx: e.tensor_scalar(out=rt[:, 38:39], in0=rt[:, 61 + kx:62 + kx], scalar1=2.0, scalar2=None, op0=ALU.mult))
                dv(lambda e, kx=kx: e.tensor_scalar(out=rt[:, 39:40], in0=rt[:, 61 + kx:62 + kx], scalar1=2.0, scalar2=1.0, op0=ALU.mult, op1=ALU.add))
                dv(lambda e, kx=kx, i=i: e.tensor_copy(out=destG[:, i, 2 * kx:2 * kx + 2], in_=rt[:, 38:40]), extra_w=["dest%d" % i])
                dv(lambda e, kx=kx, i=i: e.tensor_tensor(out=wgt[:, i, kx:kx + 1], in0=rt[:, 59 + kx:60 + kx], in1=rt[:, 36 + kx:37 + kx],
                                                         op=ALU.mult), extra_w=["wgt%d" % i])
            for kx in range(2):
                P.add("pool", lambda e, xb=xb, i=i, kx=kx: e.indirect_dma_start(
                    out=xs[:, :], out_offset=bass.IndirectOffsetOnAxis(ap=destI[:, i, kx:kx + 1], axis=0),
                    in_=xb, in_offset=None, bounds_check=None, oob_is_err=False),
                    reads=[xbk, "dest%d" % i], writes=["xs"], dma="sc%d" % (i % 6))

        pre, chn = [], []
        for i in range(NB):
            P.begin_record()
            route_block(i, 0)
            pre.append(P.end_record())
            P.begin_record()
            route_block(i, 1)
            chn.append(P.end_record())
        P.replay_interleaved([pre[0], pre[1]])
        for i in range(0, NB, 2):
            if i + 2 < NB:
                P.replay_interleaved([pre[i + 2], pre[i + 3]])
            P.replay_interleaved([chn[i], chn[i + 1]])
        P.barrier()
        if stop_after == "F":
            break

        xg = [R4b[:, 8704 + s * 2048:8704 + (s + 1) * 2048].rearrange("p (b d) -> p b d", b=2) for s in range(2)]
        xgT = R4b[:, 12800:14848].rearrange("p (k t) -> p k t", k=8)
        actT = R4b[:, 14848:15872].rearrange("p (k t) -> p k t", k=4)
        ysb = [R4[:, s * 1024:(s + 1) * 1024] for s in range(2)]
        def load_xg(ex):
            q = ex % 2
            P.add("sp", lambda e: e.dma_start(out=xg[q], in_=xs[ex * CAP:(ex + 1) * CAP, :].rearrange("(b p) d -> p b d", p=128)),
                  reads=["xs"], writes=["xg%d" % q], dma="xg%d" % q)

        for ex in range(NE):
            s = ex % 3
            xs_ = ex % 2
            if ex >= 3:
                load_expert_w(ex)
            if ex == 0:
                load_xg(0)
            if ex + 1 < NE:
                load_xg(ex + 1)
            for sbk in range(2):
                for kk in range(2):
                    b = bank()
                    for j in range(4):
                        k = kk * 4 + j
                        P.add("pe", lambda e, b=b, j=j, k=k, xs_=xs_, sbk=sbk: e.transpose(
                            out=ps[b][:, :].bitcast(BF16)[:, j * 128:(j + 1) * 128], in_=xg[xs_][:, sbk, k * 128:(k + 1) * 128], identity=identb[:]),
                            reads=["xg%d" % xs_, "identb"], writes=["ps%d" % b])
                    if (sbk + kk) % 2 == 0:
                        P.add("dve", lambda e, b=b, kk=kk, sbk=sbk: e.tensor_copy(
                            out=xgT[:, kk * 4:(kk + 1) * 4, sbk * 128:(sbk + 1) * 128],
                            in_=ps[b][:, :].bitcast(BF16)[:, 0:512].rearrange("p (a t) -> p a t", a=4)),
                            reads=["ps%d" % b], writes=["xgT"])
                    else:
                        P.add("act", lambda e, b=b, kk=kk, sbk=sbk: e.copy(
                            out=xgT[:, kk * 4:(kk + 1) * 4, sbk * 128:(sbk + 1) * 128],
                            in_=ps[b][:, :].bitcast(BF16)[:, 0:512].rearrange("p (a t) -> p a t", a=4)),
                            reads=["ps%d" % b], writes=["xgT"])
            for hc in range(4):
                b = bank()
                for which, Wm, wkey in ((0, Wg, "Wg%d" % s), (1, Wu, "Wu%d" % s)):
                    for k in range(8):
                        P.add("pe", lambda e, b=b, k=k, hc=hc, which=which, Wm=Wm, s=s: e.matmul(
                            ps[b][:, which * 256:(which + 1) * 256], lhsT=Wm[s][:, k, hc * 128:(hc + 1) * 128], rhs=xgT[:, k, :],
                            start=(k == 0), stop=(k == 7)),
                            reads=[wkey, "xgT"], writes=["ps%d" % b])
                sq = scq[hc % 2]
                P.add("act", lambda e, b=b, sq=sq: e.activation(out=sq[:, 0:256], in_=ps[b][:, 0:256], func=AF.Silu),
                      reads=["ps%d" % b], writes=["scq%d" % (hc % 2)])
                P.add("dve", lambda e, b=b, sq=sq, hc=hc: e.tensor_tensor(out=actT[:, hc, :], in0=sq[:, 0:256], in1=ps[b][:, 256:512], op=ALU.mult),
                      reads=["ps%d" % b, "scq%d" % (hc % 2)], writes=["actT"])
            for sbk in range(2):
                for nt in range(2):
                    b = bank()
                    for hc in range(4):
                        P.add("pe", lambda e, b=b, hc=hc, sbk=sbk, nt=nt, s=s: e.matmul(
                            ps[b][:, :], lhsT=actT[:, hc, sbk * 128:(sbk + 1) * 128], rhs=Wd[s][:, hc, nt * 512:(nt + 1) * 512],
                            start=(hc == 0), stop=(hc == 3)),
                            reads=["actT", "Wd%d" % s], writes=["ps%d" % b])
                    if nt == 0:
                        P.add("act", lambda e, b=b, sbk=sbk: e.copy(out=ysb[sbk][:, 0:512], in_=ps[b][:, :]),
                              reads=["ps%d" % b], writes=["ysb%d" % sbk])
                    else:
                        P.add("dve", lambda e, b=b, sbk=sbk: e.tensor_copy(out=ysb[sbk][:, 512:1024], in_=ps[b][:, :]),
                              reads=["ps%d" % b], writes=["ysb%d" % sbk])
                P.add("sp", lambda e, ex=ex, sbk=sbk: e.dma_start(out=ys[(ex * CAP + sbk * 128) * 2:(ex * CAP + (sbk + 1) * 128) * 2, :].rearrange("(p h) c -> p (h c)", h=2), in_=ysb[sbk]),
                      reads=["ysb%d" % sbk], writes=["ys"], dma="ys%d" % sbk)

        P.barrier()
        R1f = R1[:, :].bitcast(F32)
        Ybuf = [[R1f[:, (2 * q + kx) * 1024:(2 * q + kx + 1) * 1024] for kx in range(2)] for q in range(2)]

        def combine_block(i):
            xk = "X%d" % i
            q = i % 2
            y12 = Ybuf[q]
            for kx in range(2):
                for hf in range(2):
                    P.add("pool", lambda e, kx=kx, hf=hf: e.indirect_dma_start(
                        out=y12[kx][:, hf * 512:(hf + 1) * 512], out_offset=None, in_=ys[:, :],
                        in_offset=bass.IndirectOffsetOnAxis(ap=destG[:, i, 2 * kx + hf:2 * kx + hf + 1], axis=0),
                        bounds_check=None, oob_is_err=False),
                        reads=["ys", "dest%d" % i], writes=["y%d_%d_%d" % (q, kx, hf)], dma="ga%d_%d_%d" % (q, kx, hf))
            ya = ["y%d_0_0" % q, "y%d_0_1" % q]
            yb = ["y%d_1_0" % q, "y%d_1_1" % q]
            P.add("dve", lambda e: e.tensor_scalar(out=y12[0], in0=y12[0], scalar1=wgt[:, i, 0:1], scalar2=None, op0=ALU.mult),
                  reads=["wgt%d" % i], writes=ya)
            P.add("dve", lambda e: e.scalar_tensor_tensor(out=y12[0], in0=y12[1], scalar=wgt[:, i, 1:2], in1=y12[0],
                                                          op0=ALU.mult, op1=ALU.add),
                  reads=["wgt%d" % i] + yb, writes=ya)
            P.add("dve", lambda e: e.scalar_tensor_tensor(out=X[:, i, :], in0=X[:, i, :], scalar=ALPHA, in1=y12[0],
                                                          op0=ALU.mult, op1=ALU.add),
                  reads=ya, writes=[xk])
            ln_block(l, i, 1)
            if l == n_layers - 1:
                P.add("sp", lambda e: e.dma_start(out=out[i, :, :], in_=X[:, i, :]), reads=[xk], writes=["out"], dma="out%d" % (i % 2))

        hrec = []
        for i in range(NB):
            P.begin_record()
            combine_block(i)
            hrec.append(P.end_record())
        for i in range(0, NB, 2):
            P.replay_interleaved([hrec[i], hrec[i + 1]])
        P.barrier()

    if dbg is not None:
        loc = locals()
        for (dn, dfn, dshape, ddt) in dbg:
            P.add("sp", lambda e, dn=dn, dfn=dfn: e.dma_start(out=dbg_out[dn].ap(), in_=dfn(loc)), writes=["dbgout"], dma="dbg")
    P.barrier()
    with nc.Block() as block:
        P.emit(nc, block, es)
    es.close()
    return nc


RG = [[0, 1, 2, 3], [4, 5, 6, 7]]

def _col_order():
    cols = []
    for c in range(4):
        cols += list(range(c * 128, (c + 1) * 128))
        cols += list(range(512 + c * 128, 512 + (c + 1) * 128))
    perm = np.array([(p // 64) * 64 + ((p % 64) + 32) % 64 for p in range(128)])
    for h in range(4):
        for off in (1024, 1536):
            base = off + h * 128
            cols += list(base + np.arange(128))
            cols += list(base + perm)
    cols += list(range(2048, 2560))
    return np.array(cols, dtype=np.int64)


_COLS = _col_order()


def _host_inputs(inp):
    f32 = np.float32
    x = np.asarray(inp["x"], f32)
    w_in = np.asarray(inp["w_in"], f32)[:, :, _COLS]
    b_in = np.asarray(inp["b_in"], f32)[:, _COLS]
    shared = {}
    shared["w_in"] = np.ascontiguousarray(w_in)
    shared["bfm"] = np.ascontiguousarray(b_in[:, :3072].reshape(DEPTH, 24, 128).transpose(0, 2, 1))
    shared["bv"] = np.ascontiguousarray(np.broadcast_to(b_in[:, None, 3072:], (DEPTH, 128, 512)))
    cw = np.asarray(inp["conv_w"], f32)[:, :, 0, :]
    convp = np.zeros((DEPTH, 128, 4, 34), f32)
    convp[:, :, :, 0:31] = cw.reshape(DEPTH, 31, 4, 128).transpose(0, 3, 2, 1)
    convp[:, :, :, 31] = np.asarray(inp["conv_b"], f32).reshape(DEPTH, 4, 128).transpose(0, 2, 1)
    convp[:, :, :, 32] = np.asarray(inp["conv_ln_g"], f32).reshape(DEPTH, 4, 128).transpose(0, 2, 1)
    convp[:, :, :, 33] = np.asarray(inp["conv_ln_b"], f32).reshape(DEPTH, 4, 128).transpose(0, 2, 1)
    shared["convp"] = convp
    lam = np.stack([np.asarray(inp[k], f32) for k in ("lam_q1", "lam_k1", "lam_q2", "lam_k2")], axis=1)
    shared["lamv"] = np.ascontiguousarray(np.broadcast_to(lam[:, None], (DEPTH, 128, 4, 64)))
    shared["gsub"] = np.ascontiguousarray(np.broadcast_to(np.asarray(inp["subln_g"], f32)[:, None, :], (DEPTH, 128, 128)))
    shared["w_out"] = np.ascontiguousarray(np.asarray(inp["w_out"], f32))
    lnp = np.stack([np.asarray(inp[k], f32) for k in ("ln1_g", "ln1_b", "ln2_g", "ln2_b")], axis=1)
    shared["lnp"] = np.ascontiguousarray(np.broadcast_to(lnp[:, :, None, :], (DEPTH, 4, 128, D)))
    shared["w_r"] = np.ascontiguousarray(np.concatenate([np.asarray(inp["w_rg"], f32), np.asarray(inp["w_re"], f32)], axis=-1))
    b_r = np.concatenate([np.asarray(inp["b_rg"], f32), np.asarray(inp["b_re"], f32)], axis=-1)
    shared["b_r"] = np.ascontiguousarray(np.broadcast_to(b_r[:, None, :], (DEPTH, 128, 36)))
    shared["w_gate_e"] = np.ascontiguousarray(np.asarray(inp["w_gate_e"], f32))
    shared["w_up_e"] = np.ascontiguousarray(np.asarray(inp["w_up_e"], f32))
    shared["w_down_e"] = np.ascontiguousarray(np.asarray(inp["w_down_e"], f32))
    shared["ident"] = np.eye(128, dtype=f32)
    shared["tri"] = np.triu(np.ones((128, 128), f32), k=1)
    shared["ec"] = np.ascontiguousarray(np.broadcast_to((np.arange(NE, dtype=f32) * CAP)[None, :], (128, NE)))
    half = 32
    inv_freq = (1.0 / (np.float32(10000.0) ** (np.arange(half, dtype=f32) * f32(2.0) / f32(64)))).astype(f32)
    maps = []
    for c in range(8):
        b, r = c // 4, c % 4
        m = dict(shared)
        blocks = np.arange(NB) * 4 + r
        m["x"] = np.ascontiguousarray(x[b].reshape(64, 128, D)[blocks])
        pos = (blocks[:, None] * 128 + np.arange(128)[None, :]).reshape(-1).astype(f32)
        ang = (pos[:, None] * inv_freq[None, :]).astype(f32)
        cos, sin = np.cos(ang).astype(f32), np.sin(ang).astype(f32)
        cos64 = np.concatenate([cos, cos], axis=1).T
        sin64 = np.concatenate([-sin, sin], axis=1).T
        m["cosT"] = np.ascontiguousarray(np.concatenate([cos64, cos64], axis=0))
        m["sinT"] = np.ascontiguousarray(np.concatenate([sin64, sin64], axis=0))
        mb = np.zeros((128, 4, 128), f32)
        for q in range(4):
            if q > r:
                mb[:, q, :] = NEG
            elif q == r:
                mb[64:, q, :64] = NEG
        m["maskb"] = mb
        hc = np.zeros((128, 4), f32)
        hc[:, r] = 1.0
        m["hcoef"] = hc
        maps.append(m)
    return maps


_NC_CACHE = {}


def kernel(**inputs):
    maps = _host_inputs(inputs)
    if "nc" not in _NC_CACHE:
        _NC_CACHE["nc"] = build()
    nc = _NC_CACHE["nc"]
    res = run_bass_kernel_spmd(nc, maps, core_ids=list(range(8)))
    outp = np.zeros((2, 64, 128, D), np.float32)
    for c in range(8):
        b, r = c // 4, c % 4
        outp[b, np.arange(NB) * 4 + r] = np.asarray(res.results[c]["out"], np.float32)
    return outp.reshape(2, 8192, D)
```
